# Optimizing a Trainium2 kernel written in Bass

```python
import jax
import jax.numpy as jnp
from jax import lax
import numpy as np

D_MODEL = 1024
BATCH = 4
SEQ = 8192
DEPTH = 2

CHUNK = 64
POOL_WIDTH = D_MODEL // 4
POOL_WINDOWS = (2, 4, 8, 16)
POOL_GROUP_DIM = POOL_WIDTH // len(POOL_WINDOWS)
CONV_WIDTH = D_MODEL // 4
CONV_KERNEL = 31
RWKV_WIDTH = D_MODEL - POOL_WIDTH - CONV_WIDTH
RWKV_HEAD_DIM = 64
RWKV_HEADS = RWKV_WIDTH // RWKV_HEAD_DIM
DECAY_RANK = 64
ICLR_RANK = 64
GATE_RANK = 128
VRES_RANK = 32
N_RWKV_COLS = 3 * RWKV_WIDTH + DECAY_RANK + ICLR_RANK + GATE_RANK
N_IN = POOL_WIDTH + 2 * CONV_WIDTH + N_RWKV_COLS
D_FF = ((8 * D_MODEL // 3 + 255) // 256) * 256
N_EXPERTS = 8
TOP_K = 2
D_FF_EXPERT = 7 * D_MODEL // 2
MOE_BLOCK = 256
N_DENSE = (DEPTH + 1) // 2
N_MOE = DEPTH // 2
RMS_EPS = 1e-6
LN_EPS = 1e-5
GN_EPS = 64e-5

kernel_name = 'hybrid_pool_conv_rwkv7_moe_block'


def rmsnorm(x, g):
    xf = x.astype(jnp.float32)
    y = xf * lax.rsqrt(jnp.mean(xf * xf, axis=-1, keepdims=True) + RMS_EPS)
    return (y * g.astype(jnp.float32)).astype(x.dtype)


def pool_mixer(u, w, scale):
    bsz, t, _ = u.shape
    uf = u.astype(jnp.float32)
    csum = jnp.pad(jnp.cumsum(uf, axis=1), ((0, 0), (1, 0), (0, 0)))
    pos = jnp.arange(1, t + 1, dtype=jnp.float32)[None, :, None]
    groups = []
    for gi, win in enumerate(POOL_WINDOWS):
        sl = slice(gi * POOL_GROUP_DIM, (gi + 1) * POOL_GROUP_DIM)
        c = csum[..., sl]
        lower = jnp.pad(c[:, : t - win + 1], ((0, 0), (win - 1, 0), (0, 0)))
        trailing_mean = (c[:, 1:] - lower) / jnp.minimum(pos, float(win))
        groups.append(trailing_mean - uf[..., sl])
    d = jnp.stack(groups, axis=2)
    y = jnp.einsum('btgc,gcd->btgd', d, w.astype(jnp.float32)).reshape(bsz, t, POOL_WIDTH)
    return (y * scale.astype(jnp.float32)).astype(u.dtype)


def conv_module(p, w, b, ln_g, ln_b):
    u = p[..., :CONV_WIDTH] * jax.nn.sigmoid(p[..., CONV_WIDTH:])
    y = lax.conv_general_dilated(u, w[:, None, :].astype(u.dtype), window_strides=(1,), padding=[(CONV_KERNEL - 1, 0)], dimension_numbers=('NWC', 'WIO', 'NWC'), feature_group_count=CONV_WIDTH)
    yf = y.astype(jnp.float32) + b.astype(jnp.float32)
    mu = jnp.mean(yf, axis=-1, keepdims=True)
    var = jnp.mean(jnp.square(yf - mu), axis=-1, keepdims=True)
    yf = (yf - mu) * lax.rsqrt(var + LN_EPS) * ln_g.astype(jnp.float32) + ln_b.astype(jnp.float32)
    return jax.nn.silu(yf).astype(p.dtype)


def rwkv7_scan(r, decay, k, v, kk, a):
    bsz, t, h, n = r.shape

    def to_chunks(z):
        return jnp.moveaxis(z, 1, 0).reshape(t // CHUNK, CHUNK, bsz, h, n)

    def frame_step(state, inp):
        r_t, w_t, k_t, v_t, kk_t, a_t = inp
        sa = jnp.einsum('bhvk,bhk->bhv', state, -kk_t)
        state = state * w_t[:, :, None, :] + sa[..., None] * (kk_t * a_t)[:, :, None, :] + v_t[..., None] * k_t[:, :, None, :]
        return state, jnp.einsum('bhvk,bhk->bhv', state, r_t)

    def chunk_step(state, chunk_inp):
        return lax.scan(frame_step, state, chunk_inp)

    s0 = jnp.zeros((bsz, h, n, n), jnp.float32)
    _, y = lax.scan(chunk_step, s0, tuple(to_chunks(z) for z in (r, decay, k, v, kk, a)))
    return jnp.moveaxis(y.reshape(t, bsz, h, n), 0, 1)


def rwkv7_mixer(c, mu, w0, w2, a0, a2, g2, k_k, k_a, r_k, gn_g, gn_b, v_first, vres):
    f32 = jnp.float32
    bsz, t, _ = c.shape
    cf = c.astype(f32)
    prev = jnp.pad(cf, ((0, 0), (1, 0), (0, 0)))[:, :-1]
    cf = cf + mu.astype(f32) * (prev - cf)
    wd = RWKV_WIDTH
    r = cf[..., :wd]
    k = cf[..., wd:2 * wd]
    v = cf[..., 2 * wd:3 * wd]
    o = 3 * wd
    xw = cf[..., o:o + DECAY_RANK]
    o += DECAY_RANK
    xa = cf[..., o:o + ICLR_RANK]
    o += ICLR_RANK
    xg = cf[..., o:o + GATE_RANK]
    w_log = -jax.nn.softplus(-(w0.astype(f32) + jnp.tanh(xw) @ w2.astype(f32))) - 0.5
    decay = jnp.exp(-jnp.exp(w_log))
    a = jax.nn.sigmoid(a0.astype(f32) + xa @ a2.astype(f32))
    g = jax.nn.sigmoid(xg) @ g2.astype(f32)
    if vres is not None:
        v0, v1, v2 = vres
        v = v + (v_first - v) * jax.nn.sigmoid(v0.astype(f32) + (v @ v1.astype(f32)) @ v2.astype(f32))

    def heads(z):
        return z.reshape(bsz, t, RWKV_HEADS, RWKV_HEAD_DIM)

    kk = heads(k * k_k.astype(f32))
    kk = kk / jnp.maximum(jnp.sqrt(jnp.sum(kk * kk, axis=-1, keepdims=True)), 1e-12)
    k = k * (1.0 + (a - 1.0) * k_a.astype(f32))
    rh, kh, vh = heads(r), heads(k), heads(v)
    y = rwkv7_scan(rh, heads(decay), kh, vh, kk, heads(a))
    ym = jnp.mean(y, axis=-1, keepdims=True)
    yv = jnp.mean(jnp.square(y - ym), axis=-1, keepdims=True)
    y = ((y - ym) * lax.rsqrt(yv + GN_EPS)).reshape(bsz, t, wd) * gn_g.astype(f32) + gn_b.astype(f32)
    y = heads(y) + jnp.sum(rh * kh * r_k.astype(f32), axis=-1, keepdims=True) * vh
    y = y.reshape(bsz, t, wd) * g
    return y.astype(c.dtype), v


def swiglu(x, w_gate, w_up, w_down):
    return (jax.nn.silu(x @ w_gate) * (x @ w_up)) @ w_down


def moe_swiglu(h, router, w_gate, w_up, w_down):
    bsz, t, d = h.shape
    n = bsz * t
    xf = h.reshape(n, d)
    logits = (xf @ router).astype(jnp.float32)
    top_val, top_idx = lax.top_k(logits, TOP_K)
    gates = jax.nn.softmax(top_val, axis=-1)
    flat_e = top_idx.reshape(-1).astype(jnp.int32)
    flat_tok = jnp.arange(n * TOP_K, dtype=jnp.int32) // TOP_K
    flat_g = gates.reshape(-1)
    order = jnp.argsort(flat_e)
    e_sorted = flat_e[order]
    tok_sorted = flat_tok[order]
    g_sorted = flat_g[order]
    counts = jnp.zeros((N_EXPERTS,), jnp.int32).at[flat_e].add(1)
    starts = jnp.cumsum(counts) - counts
    padded = (counts + MOE_BLOCK - 1) // MOE_BLOCK * MOE_BLOCK
    pad_ends = jnp.cumsum(padded)
    pad_starts = pad_ends - padded
    rank = jnp.arange(n * TOP_K, dtype=jnp.int32) - starts[e_sorted]
    dest = pad_starts[e_sorted] + rank
    n_blocks = (n * TOP_K + MOE_BLOCK - 1) // MOE_BLOCK + N_EXPERTS
    n_rows = n_blocks * MOE_BLOCK
    x_disp = jnp.zeros((n_rows, d), h.dtype).at[dest].set(xf[tok_sorted])
    block_e = jnp.minimum(jnp.searchsorted(pad_ends, jnp.arange(n_blocks, dtype=jnp.int32) * MOE_BLOCK, side='right'), N_EXPERTS - 1)

    def block_fn(args):
        xb, e = args
        return swiglu(xb, w_gate[e], w_up[e], w_down[e])

    y_disp = lax.map(block_fn, (x_disp.reshape(n_blocks, MOE_BLOCK, d), block_e)).reshape(n_rows, d)
    y = jnp.zeros((n, d), h.dtype).at[tok_sorted].add(y_disp[dest] * g_sorted[:, None].astype(h.dtype))
    return y.reshape(bsz, t, d)


def setup_inputs(seed: int = 0) -> dict:
    key = jax.random.key(seed)
    ks = iter(jax.random.split(key, 40))

    def nrm(shape, s):
        return jax.random.normal(next(ks), shape, jnp.float32) * s

    def uni(shape, lo, hi):
        return jax.random.uniform(next(ks), shape, jnp.float32, lo, hi)

    L = DEPTH
    LV = DEPTH - 1
    return {
        'x': nrm((BATCH, SEQ, D_MODEL), 1.0),
        'norm1_g': 1.0 + nrm((L, D_MODEL), 0.02),
        'w_in': nrm((L, D_MODEL, N_IN), D_MODEL ** -0.5),
        'pool_w': nrm((L, len(POOL_WINDOWS), POOL_GROUP_DIM, POOL_GROUP_DIM), POOL_GROUP_DIM ** -0.5),
        'pool_scale': 1.0 + nrm((L, POOL_WIDTH), 0.1),
        'conv_w': nrm((L, CONV_KERNEL, CONV_WIDTH), CONV_KERNEL ** -0.5),
        'conv_b': nrm((L, CONV_WIDTH), 0.02),
        'conv_ln_g': 1.0 + nrm((L, CONV_WIDTH), 0.02),
        'conv_ln_b': nrm((L, CONV_WIDTH), 0.02),
        'shift_mu': uni((L, N_RWKV_COLS), 0.0, 1.0),
        'rwkv_w0': uni((L, RWKV_WIDTH), -4.0, 0.0),
        'rwkv_w2': nrm((L, DECAY_RANK, RWKV_WIDTH), 0.5 * DECAY_RANK ** -0.5),
        'rwkv_a0': nrm((L, RWKV_WIDTH), 0.1),
        'rwkv_a2': nrm((L, ICLR_RANK, RWKV_WIDTH), 0.5 * ICLR_RANK ** -0.5),
        'rwkv_g2': nrm((L, GATE_RANK, RWKV_WIDTH), GATE_RANK ** -0.5),
        'rwkv_k_k': 0.85 + nrm((L, RWKV_WIDTH), 0.05),
        'rwkv_k_a': 1.0 + nrm((L, RWKV_WIDTH), 0.05),
        'rwkv_r_k': nrm((L, RWKV_HEADS, RWKV_HEAD_DIM), 0.1),
        'rwkv_gn_g': 1.0 + nrm((L, RWKV_WIDTH), 0.02),
        'rwkv_gn_b': nrm((L, RWKV_WIDTH), 0.02),
        'rwkv_v0': 1.0 + nrm((LV, RWKV_WIDTH), 0.1),
        'rwkv_v1': nrm((LV, RWKV_WIDTH, VRES_RANK), RWKV_WIDTH ** -0.5),
        'rwkv_v2': nrm((LV, VRES_RANK, RWKV_WIDTH), 0.5 * VRES_RANK ** -0.5),
        'w_out': nrm((L, D_MODEL, D_MODEL), D_MODEL ** -0.5),
        'norm2_g': 1.0 + nrm((L, D_MODEL), 0.02),
        'ffn_w_gate': nrm((N_DENSE, D_MODEL, D_FF), D_MODEL ** -0.5),
        'ffn_w_up': nrm((N_DENSE, D_MODEL, D_FF), D_MODEL ** -0.5),
        'ffn_w_down': nrm((N_DENSE, D_FF, D_MODEL), D_FF ** -0.5),
        'moe_router': nrm((N_MOE, D_MODEL, N_EXPERTS), D_MODEL ** -0.5),
        'moe_w_gate': nrm((N_MOE, N_EXPERTS, D_MODEL, D_FF_EXPERT), D_MODEL ** -0.5),
        'moe_w_up': nrm((N_MOE, N_EXPERTS, D_MODEL, D_FF_EXPERT), D_MODEL ** -0.5),
        'moe_w_down': nrm((N_MOE, N_EXPERTS, D_FF_EXPERT, D_MODEL), D_FF_EXPERT ** -0.5),
        'final_g': 1.0 + nrm((D_MODEL,), 0.02),
    }


def reference(x, norm1_g, w_in, pool_w, pool_scale, conv_w, conv_b, conv_ln_g, conv_ln_b, shift_mu, rwkv_w0, rwkv_w2, rwkv_a0, rwkv_a2, rwkv_g2, rwkv_k_k, rwkv_k_a, rwkv_r_k, rwkv_gn_g, rwkv_gn_b, rwkv_v0, rwkv_v1, rwkv_v2, w_out, norm2_g, ffn_w_gate, ffn_w_up, ffn_w_down, moe_router, moe_w_gate, moe_w_up, moe_w_down, final_g):
    h = x
    v_first = None
    for l in range(DEPTH):
        hn = rmsnorm(h, norm1_g[l])
        proj = hn @ w_in[l]
        p_pool = proj[..., :POOL_WIDTH]
        p_conv = proj[..., POOL_WIDTH:POOL_WIDTH + 2 * CONV_WIDTH]
        p_rwkv = proj[..., POOL_WIDTH + 2 * CONV_WIDTH:]
        y_pool = pool_mixer(p_pool, pool_w[l], pool_scale[l])
        y_conv = conv_module(p_conv, conv_w[l], conv_b[l], conv_ln_g[l], conv_ln_b[l])
        vres = None if l == 0 else (rwkv_v0[l - 1], rwkv_v1[l - 1], rwkv_v2[l - 1])
        y_rwkv, v_l = rwkv7_mixer(p_rwkv, shift_mu[l], rwkv_w0[l], rwkv_w2[l], rwkv_a0[l], rwkv_a2[l], rwkv_g2[l], rwkv_k_k[l], rwkv_k_a[l], rwkv_r_k[l], rwkv_gn_g[l], rwkv_gn_b[l], v_first, vres)
        if l == 0:
            v_first = v_l
        h = h + jnp.concatenate([y_pool, y_conv, y_rwkv], axis=-1) @ w_out[l]
        hn = rmsnorm(h, norm2_g[l])
        i = l // 2
        if l % 2 == 0:
            h = h + swiglu(hn, ffn_w_gate[i], ffn_w_up[i], ffn_w_down[i])
        else:
            h = h + moe_swiglu(hn, moe_router[i], moe_w_gate[i], moe_w_up[i], moe_w_down[i])
    return rmsnorm(h, final_g)
```

```python
from contextlib import ExitStack
import numpy as np
import concourse.bass as bass
import concourse.mybir as mybir
from concourse.bass_utils import run_bass_kernel_spmd

F32 = mybir.dt.float32
BF16 = mybir.dt.bfloat16
AF = mybir.ActivationFunctionType
ALU = mybir.AluOpType
AX = mybir.AxisListType

D = 1024
NIN = 2560
DFF = 2816
DFE = 3584
NE = 8
SEQ = 8192
NDMA_SLOTS = 8
_KSTOP = None
_KSKIP = ()


class _Stop(Exception):
    pass


def chk(tag):
    if _KSTOP == tag:
        raise _Stop()
V, A, G, T, SY = "vector", "scalar", "gpsimd", "tensor", "sync"
CDEC = 0.6065306597126334


class Prog:
    def __init__(self, nc):
        self.nc = nc
        self.ops = []
        self.last_write = {}
        self.readers = {}
        self.stacks = [ExitStack()]
        self.ntile = 0
        self.dnext = {SY: 0, A: 0, G: 0}
        self.last_on = {}
        self.bar = set()
        self.base = {}

    def scope(self):
        prog = self

        class _S:
            def __enter__(s):
                prog.stacks.append(ExitStack())

            def __exit__(s, *a):
                prog.barrier()
                prog.stacks.pop().close()
                return False
        return _S()

    def sb(self, shape, dtype, name):
        self.ntile += 1
        self.base[f"{name}_{self.ntile}"] = name
        return self.stacks[-1].enter_context(self.nc.sbuf_tensor(f"{name}_{self.ntile}", list(shape), dtype))

    def ps(self, shape, dtype, name):
        self.ntile += 1
        self.base[f"{name}_{self.ntile}"] = name
        return self.stacks[-1].enter_context(self.nc.psum_tensor(f"{name}_{self.ntile}", list(shape), dtype))

    def _k(self, x):
        if isinstance(x, (str, tuple)):
            return x
        return self.base.get(x.name, x.name)

    def barrier(self):
        self.bar = set(self.last_on.values())

    def op(self, eng, fn, reads=(), writes=(), dma=False):
        i = len(self.ops)
        deps = set(self.bar)
        reads = [self._k(x) for x in reads]
        writes = [self._k(x) for x in writes]
        if dma:
            slot = self.dnext[eng]
            self.dnext[eng] = (slot + 1) % NDMA_SLOTS
            semkey = (eng, slot)
        else:
            semkey = eng
        for k in reads:
            j = self.last_write.get(k)
            if j is not None:
                deps.add(j)
            if isinstance(k, str) and k.startswith("bank"):
                deps.update(v for sk, v in self.readers.get(k, {}).items() if sk != semkey)
        for k in writes:
            j = self.last_write.get(k)
            if j is not None:
                deps.add(j)
            deps.update(self.readers.get(k, {}).values())
        for k in reads:
            self.readers.setdefault(k, {})[semkey] = i
        for k in writes:
            self.last_write[k] = i
            self.readers[k] = {}
        self.last_on[semkey] = i
        self.ops.append((eng, fn, deps, semkey))
        return i

    def dma(self, q, out, in_, reads=None, writes=None, **kw):
        return self.op(q, lambda e: e.dma_start(out=out, in_=in_, **kw),
                       [in_] if reads is None else reads, [out] if writes is None else writes, dma=True)

    def tt(self, eng, out, in0, in1, op, r=None, w=None):
        return self.op(eng, lambda e: e.tensor_tensor(out=out, in0=in0, in1=in1, op=op),
                       [in0, in1] if r is None else r, [out] if w is None else w)

    def ts(self, eng, out, in0, s1, s2=None, op0=ALU.mult, op1=None, r=None, w=None):
        rr = [in0] + [x for x in (s1, s2) if not isinstance(x, (int, float, type(None)))]
        kw = {} if op1 is None else {"op1": op1}
        return self.op(eng, lambda e: e.tensor_scalar(out=out, in0=in0, scalar1=s1, scalar2=s2, op0=op0, **kw),
                       rr if r is None else r, [out] if w is None else w)

    def stt(self, out, in0, scalar, in1, op0, op1, r=None, w=None):
        rr = [in0, in1] + ([] if isinstance(scalar, (int, float)) else [scalar])
        return self.op(V, lambda e: e.scalar_tensor_tensor(out=out, in0=in0, scalar=scalar, in1=in1, op0=op0, op1=op1),
                       rr if r is None else r, [out] if w is None else w)

    def act(self, out, in_, func, bias=None, scale=1.0, accum_out=None, r=None, w=None):
        rr = [in_] + [x for x in (bias, scale) if not isinstance(x, (int, float, type(None)))]
        ww = [out] + ([accum_out] if accum_out is not None else [])
        kw = {}
        if bias is not None:
            kw["bias"] = bias
        if accum_out is not None:
            kw["accum_out"] = accum_out
        return self.op(A, lambda e: e.activation(out=out, in_=in_, func=func, scale=scale, **kw),
                       rr if r is None else r, ww if w is None else w)

    def cp(self, eng, out, in_, r=None, w=None):
        if eng == A:
            return self.act(out, in_, AF.Copy, r=r, w=w)
        return self.op(eng, lambda e: e.tensor_copy(out=out, in_=in_), [in_] if r is None else r, [out] if w is None else w)

    def mm(self, out, lhsT, rhs, start=True, stop=True, r=None, w=None):
        return self.op(T, lambda e: e.matmul(out, lhsT=lhsT, rhs=rhs, start=start, stop=stop),
                       [lhsT, rhs] if r is None else r, [out] if w is None else w)

    def tr(self, out, in_, ident, r=None, w=None):
        return self.op(T, lambda e: e.transpose(out=out, in_=in_, identity=ident),
                       [in_, ident] if r is None else r, [out] if w is None else w)

    def memset(self, eng, ap, val):
        return self.op(eng, lambda e: e.memset(ap, val), [], [ap])

    def emit(self, final_wait_ops):
        nc = self.nc
        engs = [SY, A, V, G, T]
        with ExitStack() as st:
            csem = {e: st.enter_context(nc.semaphore(f"c_{e}")) for e in engs if e != SY}
            dsem = {(e, i): st.enter_context(nc.semaphore(f"d_{e}{i}")) for e in (SY, A, G) for i in range(NDMA_SLOTS)}
            cnt = {}
            token = [None] * len(self.ops)
            prev = [None] * len(self.ops)
            per_eng = {e: [] for e in engs}
            for i, (eng, fn, deps, semkey) in enumerate(self.ops):
                c = cnt.get(semkey, 0)
                if isinstance(semkey, tuple):
                    prev[i] = (dsem[semkey], c * 16)
                    token[i] = (dsem[semkey], (c + 1) * 16)
                else:
                    token[i] = (csem[semkey], c + 1)
                cnt[semkey] = c + 1
                per_eng[eng].append(i)
            block = st.enter_context(nc.Block())
            ops = self.ops

            def body(engname):
                def run(e):
                    waited = {}
                    for i in per_eng[engname]:
                        eng, fn, deps, semkey = ops[i]
                        isdma = isinstance(semkey, tuple)
                        need = {}
                        for j in deps:
                            if engname == T and ops[j][3] == T:
                                continue
                            s, v = token[j]
                            if need.get(s.num, (None, 0))[1] < v:
                                need[s.num] = (s, v)
                        if isdma and prev[i][1] > 0:
                            s, v = prev[i]
                            if need.get(s.num, (None, 0))[1] < v:
                                need[s.num] = (s, v)
                        for s, v in need.values():
                            if waited.get(s.num, 0) < v:
                                e.wait_ge(s, v)
                                waited[s.num] = v
                        ins = fn(e)
                        ins.then_inc(token[i][0], 16 if isdma else 1)
                    if engname == final_wait_ops[0]:
                        for j in list(final_wait_ops[1]) + sorted(self.last_on.values()):
                            s, v = token[j]
                            if waited.get(s.num, 0) < v:
                                e.wait_ge(s, v)
                                waited[s.num] = v
                return run

            block.sync(body(SY))
            block.scalar(body(A))
            block.vector(body(V))
            block.gpsimd(body(G))
            block.tensor(body(T))
        self.stacks[0].close()


def build(NTOK=SEQ, GT=2048, TT=128):
    nc = bass.Bass("TRN2", target_bir_lowering=False)
    NMOE = NTOK // 2
    NCH = TT // 64
    NSUB = TT // 128
    NTILE = NTOK // TT

    def din(name, shape):
        return nc.dram_tensor(name, list(shape), F32, kind="ExternalInput").ap()

    xs = din("xs", [NTOK, D])
    padmask_d = din("padmask", [128, NTOK // 128])
    pfix_d = din("pfix", [128, 2, 2, 16])
    norm1_g = din("norm1_g", [2, D])
    w_in = din("w_in", [2, D, NIN])
    pool_w = din("pool_w", [2, 4, 64, 64])
    pool_scale = din("pool_scale", [2, 256])
    conv_w = din("conv_w", [2, 31, 256])
    conv_b = din("conv_b", [2, 256])
    conv_ln_g = din("conv_ln_g", [2, 256])
    conv_ln_b = din("conv_ln_b", [2, 256])
    shift_mu = din("shift_mu", [2, 1792])
    rwkv_w0 = din("rwkv_w0", [2, 512])
    rwkv_w2 = din("rwkv_w2", [2, 64, 512])
    rwkv_a0 = din("rwkv_a0", [2, 512])
    rwkv_a2 = din("rwkv_a2", [2, 64, 512])
    rwkv_g2 = din("rwkv_g2", [2, 128, 512])
    rwkv_k_k = din("rwkv_k_k", [2, 512])
    rwkv_k_a = din("rwkv_k_a", [2, 512])
    rwkv_r_k = din("rwkv_r_k", [2, 512])
    rwkv_gn_g = din("rwkv_gn_g", [2, 512])
    rwkv_gn_b = din("rwkv_gn_b", [2, 512])
    rwkv_v0 = din("rwkv_v0", [1, 512])
    rwkv_v1 = din("rwkv_v1", [1, 512, 32])
    rwkv_v2 = din("rwkv_v2", [1, 32, 512])
    w_out = din("w_out", [2, D, D])
    norm2_g = din("norm2_g", [2, D])
    ffn_w_gate = din("ffn_w_gate", [1, D, DFF])
    ffn_w_up = din("ffn_w_up", [1, D, DFF])
    ffn_w_down = din("ffn_w_down", [1, DFF, D])
    moe_router = din("moe_router", [1, D, NE])
    small = _KSTOP is not None
    moe_w_gate = din("moe_w_gate", [1, NE, D, 8 if small else DFE])
    moe_w_up = din("moe_w_up", [1, NE, D, 8 if small else DFE])
    moe_w_down = din("moe_w_down", [1, NE, 8 if small else DFE, D])
    final_g = din("final_g", [D])
    ident_d = din("c_ident", [128, 128])
    maskar_d = din("c_maskar", [64, 128])
    masklow_d = din("c_masklow", [64, 64])
    i8_d = din("c_i8", [64, 64])
    cmask_d = din("c_cmask", [128, TT])
    blockones_d = din("c_blockones", [128, 128])
    headsel_d = din("c_headsel", [128, 4, 8])
    out_d = nc.dram_tensor("out", [NMOE, D], F32, kind="ExternalOutput").ap()
    hA = nc.dram_tensor("hA", [NTOK, D], F32).ap()
    hB = nc.dram_tensor("hB", [NTOK, D], F32).ap()
    vfirst = nc.dram_tensor("vfirst", [512, NTOK], F32).ap()

    P = Prog(nc)
    out_ops = []

    identf = P.sb([128, 128], F32, "identf")
    identb = P.sb([128, 128], BF16, "identb")
    P.dma(SY, identf[:], ident_d)
    P.cp(V, identb[:], identf[:])
    padmask = P.sb([128, NTOK // 128], F32, "padmask")
    P.dma(SY, padmask[:], padmask_d)
    banks = [P.ps([128, 512], F32, f"bank{i}") for i in range(8)]

    def bview(bank_i, dtype, pattern=None, **kw):
        ap = banks[bank_i][:]
        if dtype is BF16:
            ap = ap.bitcast(BF16)
        if pattern:
            ap = ap.rearrange(pattern, **kw)
        return ap

    def mixer(l, src, dst, first_out_tile):
        with P.scope():
            WIN = P.sb([128, 8, NIN], BF16, "WIN")
            for c in range(8):
                for c0 in range(0, NIN, 640):
                    P.dma(G, WIN[:, c, c0:c0 + 640], w_in[l, c * 128:(c + 1) * 128, c0:c0 + 640], writes=[("WIN", c)])
            WINk = [("WIN", c) for c in range(8)]
            WOUT = P.sb([128, 8, D], BF16, "WOUT")
            for c in range(8):
                P.dma(G, WOUT[:, c, :], w_out[l, c * 128:(c + 1) * 128, :], writes=["WOUT"])
            G1 = P.sb([128, D], F32, "G1")
            P.dma(SY, G1[:], norm1_g[l].partition_broadcast(128))
            GNG = P.sb([64, 512], F32, "GNG")
            GNB = P.sb([64, 512], F32, "GNB")
            P.dma(SY, GNG[:], rwkv_gn_g[l].partition_broadcast(64))
            P.dma(SY, GNB[:], rwkv_gn_b[l].partition_broadcast(64))
            PV = P.sb([128, 64], F32, "PV")
            PVR = P.sb([64, 128], F32, "PVR")
            MU, OMU, W0, A0, KKc, KAc, OMKA, V0c, RKc, PSC, CB, LNG, LNB = 0, 14, 28, 32, 36, 40, 44, 48, 52, 56, 58, 60, 62
            P.memset(V, PVR[:], 0.0)

            def pvec(col, src_ap, n):
                P.dma(SY, PVR[col:col + n, :], src_ap.rearrange("(c p) -> c p", p=128), writes=[PVR])
            pvec(MU, shift_mu[l], 14)
            pvec(W0, rwkv_w0[l], 4)
            pvec(A0, rwkv_a0[l], 4)
            pvec(KKc, rwkv_k_k[l], 4)
            pvec(KAc, rwkv_k_a[l], 4)
            if l == 1:
                pvec(V0c, rwkv_v0[0], 4)
            pvec(RKc, rwkv_r_k[l], 4)
            pvec(PSC, pool_scale[l], 2)
            pvec(CB, conv_b[l], 2)
            pvec(LNG, conv_ln_g[l], 2)
            pvec(LNB, conv_ln_b[l], 2)
            P.tr(banks[0][:, 0:64], PVR[:], identf[0:64, 0:64])
            P.cp(V, PV[:], banks[0][:, 0:64])
            P.ts(V, PV[:, OMU:OMU + 14], PV[:, MU:MU + 14], -1.0, 1.0, ALU.mult, ALU.add)
            P.ts(V, PV[:, OMKA:OMKA + 4], PV[:, KAc:KAc + 4], -1.0, 1.0, ALU.mult, ALU.add)
            CW = P.sb([128, 2, 31], F32, "CW")
            CWR = P.sb([31, 256], F32, "CWR")
            P.dma(SY, CWR[:], conv_w[l])
            for t in range(2):
                P.tr(banks[1][:, t * 32:t * 32 + 31], CWR[:, t * 128:(t + 1) * 128], identf[0:31, 0:31])
            P.cp(V, CW[:], banks[1][:, 0:64].rearrange("p (t j) -> p t j", j=32)[:, :, 0:31])
            DIAG = P.sb([128, 2, 31, 128], BF16, "DIAG")
            for t in range(2):
                for j in range(31):
                    P.ts(V if j % 2 else G, DIAG[:, t, j, :], identf[:], CW[:, t, j:j + 1], None, ALU.mult)
            PWf = P.sb([128, 2, 128], F32, "PWf")
            PW = P.sb([128, 2, 128], BF16, "PW")
            P.memset(V, PWf[:], 0.0)
            for t in range(2):
                P.dma(SY, PWf[0:64, t, 0:64], pool_w[l, 2 * t], writes=[PWf])
                P.dma(SY, PWf[64:128, t, 64:128], pool_w[l, 2 * t + 1], writes=[PWf])
            P.cp(V, PW[:], PWf[:])
            PFIX = P.sb([128, 2, 2, 16], F32, "PFIX")
            P.dma(SY, PFIX[:], pfix_d)
            W2A2 = P.sb([128, 512], BF16, "W2A2")
            P.dma(G, W2A2[0:64, :], rwkv_w2[l], writes=[W2A2])
            P.dma(G, W2A2[64:128, :], rwkv_a2[l], writes=[W2A2])
            G2 = P.sb([128, 512], BF16, "G2")
            P.dma(G, G2[:], rwkv_g2[l])
            if l == 1:
                V1 = P.sb([128, 4, 32], BF16, "V1")
                P.dma(G, V1[:], rwkv_v1[0].rearrange("(c p) r -> p c r", p=128))
                V2 = P.sb([32, 512], BF16, "V2")
                P.dma(G, V2[:], rwkv_v2[0])
            ONES256 = P.sb([128, 128], F32, "ONES256")
            P.memset(V, ONES256[:], 1.0 / 256.0)
            BLK = P.sb([128, 128], BF16, "BLK")
            BLKf = P.sb([128, 128], F32, "BLKf")
            P.dma(SY, BLKf[:], blockones_d)
            P.cp(V, BLK[:], BLKf[:])
            HSELf = P.sb([128, 4, 8], F32, "HSELf")
            HSEL = P.sb([128, 4, 8], BF16, "HSEL")
            P.dma(SY, HSELf[:], headsel_d)
            P.cp(V, HSEL[:], HSELf[:])
            MASKAR = P.sb([64, 128], F32, "MASKAR")
            MASKLOW = P.sb([64, 64], F32, "MASKLOW")
            I8 = P.sb([64, 64], F32, "I8")
            CMASK = P.sb([128, TT], F32, "CMASK")
            P.dma(SY, MASKAR[:], maskar_d)
            P.dma(SY, MASKLOW[:], masklow_d)
            P.dma(SY, I8[:], i8_d)
            P.dma(SY, CMASK[:], cmask_d)

            chk("m0w")
            def dbl(shape, dt, name):
                return [P.sb(shape, dt, f"{name}{b}") for b in range(2)]
            HXs = dbl([128, NSUB, D], F32, "HX")
            SQ = P.sb([128, D], F32, "SQ")
            SS = P.sb([128, NSUB], F32, "SS")
            RS = P.sb([128, NSUB], F32, "RS")
            XN = P.sb([128, NSUB, D], BF16, "XN")
            HNT = P.sb([128, 8, TT], BF16, "HNT")
            UP = P.sb([128, 2, 16 + TT], F32, "UP")
            S1 = P.sb([128, 2, 16 + TT], F32, "S1")
            S2 = P.sb([128, 2, 16 + TT], F32, "S2")
            S3 = P.sb([128, 2, 16 + TT], F32, "S3")
            S16 = P.sb([128, 16 + TT], F32, "S16")
            DP = P.sb([128, 2, TT], BF16, "DP")
            CA = P.sb([128, 2, TT], F32, "CA")
            SG = P.sb([128, TT], F32, "SG")
            U = P.sb([128, 2, 32 + TT], BF16, "U")
            YB = P.sb([128, 2, TT], F32, "YB")
            YSQ = P.sb([128, 2, TT], F32, "YSQ")
            MEAN = P.sb([128, TT], F32, "MEAN")
            VAR = P.sb([128, TT], F32, "VAR")
            RSTD = P.sb([128, TT], F32, "RSTD")
            CT = P.sb([128, TT], F32, "CT")
            HALO = P.sb([128, 14], F32, "HALO")
            TMPM = P.sb([128, TT], F32, "TMPM")
            CF = P.sb([128, 12, TT], F32, "CF")
            SGW = P.sb([128, TT], F32, "SGW")
            CUM = P.sb([128, TT], F32, "CUM")
            CUMP = P.sb([128, TT], F32, "CUMP")
            ER = P.sb([128, 4, TT], F32, "ER")
            EA = P.sb([128, TT], F32, "EA")
            EI = P.sb([128, TT], F32, "EI")
            EH = P.sb([128, TT], F32, "EH")
            AA = P.sb([128, TT], F32, "AA")
            KK = P.sb([128, TT], F32, "KK")
            KSQ = P.sb([128, TT], BF16, "KSQ")
            RN = P.sb([128, TT], F32, "RN")
            KF = P.sb([128, TT], F32, "KF")
            BETA = P.sb([128, TT], F32, "BETA")
            TM2 = P.sb([128, TT], F32, "TM2")
            VFT = P.sb([128, 4, TT], F32, "VFT") if l == 1 else None
            T1 = P.sb([32, TT], BF16, "T1") if l == 1 else None
            BH = P.sb([128, 4, TT], BF16, "BH")
            KH = P.sb([128, 4, TT], BF16, "KH")
            VB = P.sb([128, 4, TT], BF16, "VB")
            LACTs = dbl([128, 2, TT], BF16, "LACT")
            ARs = dbl([128, 4, NCH, 128], BF16, "AR")
            BTs = dbl([128, 4, TT], BF16, "BT")
            KTs = dbl([128, 4, TT], BF16, "KT")
            RKBs = dbl([128, 4, TT], BF16, "RKB")
            ARos = dbl([64, 4, NCH, 128], BF16, "ARo")
            BTos = dbl([64, 4, TT], BF16, "BTo")
            KTos = dbl([64, 4, TT], BF16, "KTo")
            ECos = dbl([64, 4, NCH], F32, "ECo")
            ECes = dbl([128, 4, NCH], F32, "ECe")
            VTOKs = dbl([64, NCH, 512], BF16, "VTOK")
            BHTs = dbl([64, NCH, 512], BF16, "BHT")
            KHTs = dbl([64, NCH, 512], BF16, "KHT")
            YMIXs = dbl([128, 8, TT], BF16, "YMIX")
            MBs = P.sb([64, 8, 128], BF16, "MBs")
            MKs = P.sb([64, 8, 128], BF16, "MKs")
            Mp = [P.sb([64, 8, 64], BF16, f"Mp{i}") for i in range(2)]
            Np = [P.sb([64, 8, 64], BF16, f"Np{i}") for i in range(2)]
            Tb = P.sb([64, 8, 64], BF16, "Tb")
            WTs = P.sb([64, 8, 64], BF16, "WTs")
            UTs = P.sb([64, 8, 64], BF16, "UTs")
            Sf = P.sb([64, 8, 64], F32, "Sf")
            Sb = P.sb([64, 8, 64], BF16, "Sb")
            YS = P.sb([64, 8, 64], F32, "YS")
            YQ = P.sb([64, 8, 64], F32, "YQ")
            ST = P.sb([64, 40], F32, "ST")
            BON = P.sb([64, 8], F32, "BON")
            YM = P.sb([64, 512], BF16, "YM")

            P.memset(V, UP[:], 0.0)
            P.memset(V, U[:], 0.0)
            P.memset(V, HALO[:], 0.0)
            P.memset(V, Sf[:], 0.0)
            P.memset(V, Sb[:], 0.0)
            for b_ in range(2):
                P.memset(G, ARs[b_][:], 0.0)
                P.memset(G, LACTs[b_][:], 0.0)

            def prep(it):
                b = it % 2
                t0 = it * TT
                lite = it < first_out_tile - 1
                HX, LACT, AR, BT, KT, RKB = HXs[b], LACTs[b], ARs[b], BTs[b], KTs[b], RKBs[b]
                ARo, BTo, KTo, ECo, ECe = ARos[b], BTos[b], KTos[b], ECos[b], ECes[b]
                VTOK, BHT, KHT, YMIX = VTOKs[b], BHTs[b], KHTs[b], YMIXs[b]
                ymn = "YMIX%d" % b
                arn, btn, ktn = "AR%d" % b, "BT%d" % b, "KT%d" % b
                P.dma(SY, HX[:], src[t0:t0 + TT, :].rearrange("(s p) d -> p s d", p=128))
                for s in range(NSUB):
                    P.act(SQ[:], HX[:, s, :], AF.Square, accum_out=SS[:, s:s + 1])
                    P.act(RS[:, s:s + 1], SS[:, s:s + 1], AF.Sqrt, bias=1e-6, scale=1.0 / D)
                    P.op(V, lambda e, s=s: e.reciprocal(out=RS[:, s:s + 1], in_=RS[:, s:s + 1]), [RS], [RS])
                    P.stt(XN[:, s, :], HX[:, s, :], RS[:, s:s + 1], G1[:], ALU.mult, ALU.mult)
                    yield
                    pT = bview(7, BF16, "p (a b) -> p a b", b=128)
                    for c in range(8):
                        P.tr(pT[:, c, :], XN[:, s, c * 128:(c + 1) * 128], identb[:], w=[banks[7]])
                    P.cp(V, HNT[:, :, s * 128:(s + 1) * 128], pT, r=[banks[7]])
                    yield

                def proj(f, pb):
                    pm = banks[pb][:, 0:TT]
                    for c in range(8):
                        P.mm(pm, WIN[:, c, f * 128:(f + 1) * 128], HNT[:, c, :], start=(c == 0), stop=(c == 7),
                             r=[("WIN", c), HNT], w=[banks[pb]])
                    return pm

                if not lite:
                    for t in range(2):
                        pm = proj(t, t % 2)
                        P.cp(A, UP[:, t, 16:16 + TT], pm, r=[banks[t % 2]], w=[UP])
                        yield
                    W_ = 16 + TT
                    P.tt(V, S1[:, :, 1:W_], UP[:, :, 1:W_], UP[:, :, 0:W_ - 1], ALU.add)
                    P.tt(G, S2[:, :, 3:W_], S1[:, :, 3:W_], S1[:, :, 1:W_ - 2], ALU.add)
                    yield
                    P.tt(V, S3[:, :, 7:W_], S2[:, :, 7:W_], S2[:, :, 3:W_ - 4], ALU.add)
                    P.tt(G, S16[:, 15:W_], S3[:, 1, 15:W_], S3[:, 1, 7:W_ - 8], ALU.add)
                    yield
                    grp = [(0, 0, S1[0:64, 0, :], 2.0), (0, 64, S2[64:128, 0, :], 4.0), (1, 0, S3[0:64, 1, :], 8.0), (1, 64, S16[64:128, :], 16.0)]
                    for (t, p0, sbuf, win) in grp:
                        for which, tile_idx in ((0, 0), (1, NTILE // 2)):
                            if it == tile_idx:
                                P.tt(V, sbuf[:, 16:32], sbuf[:, 16:32], PFIX[p0:p0 + 64, t, which, :], ALU.mult)
                        P.stt(DP[p0:p0 + 64, t, :], sbuf[:, 16:W_], 1.0 / win, UP[p0:p0 + 64, t, 16:W_], ALU.mult, ALU.subtract)
                    yield
                    P.cp(G, UP[:, :, 0:16], UP[:, :, TT:TT + 16])
                    for t in range(2):
                        pm2 = banks[t % 2][:, 0:TT]
                        P.mm(pm2, PW[:, t, :], DP[:, t, :], w=[banks[t % 2]])
                        P.act(YMIX[:, t, :], pm2, AF.Identity, scale=PV[:, PSC + t:PSC + t + 1], r=[banks[t % 2], PV], w=[(ymn, t)])
                    yield
                    for t in range(2):
                        pm = proj(2 + t, t % 2)
                        P.cp(A, CA[:, t, :], pm, r=[banks[t % 2]], w=[CA])
                        yield
                    for t in range(2):
                        pm = proj(4 + t, t % 2)
                        P.act(SG[:], pm, AF.Sigmoid, r=[banks[t % 2]])
                        P.tt(V, U[:, t, 32:32 + TT], CA[:, t, :], SG[:], ALU.mult)
                        yield
                    for t in range(2):
                        pc = banks[t % 2][:, 0:TT]
                        for j in range(31):
                            P.mm(pc, DIAG[:, t, j, :], U[:, t, 2 + j:2 + j + TT], start=(j == 0), stop=(j == 30), w=[banks[t % 2]])
                            if j % 8 == 7:
                                yield
                        P.act(YB[:, t, :], pc, AF.Identity, bias=PV[:, CB + t:CB + t + 1], r=[banks[t % 2], PV], w=[YB])
                        P.act(YSQ[:, t, :], pc, AF.Square, bias=PV[:, CB + t:CB + t + 1], r=[banks[t % 2], PV], w=[YSQ])
                        yield
                    P.cp(G, U[:, :, 0:32], U[:, :, TT:TT + 32])
                    pmean = banks[0][:, 0:TT]
                    psq = banks[1][:, 0:TT]
                    for t in range(2):
                        P.mm(pmean, ONES256[:], YB[:, t, :], start=(t == 0), stop=(t == 1), w=[banks[0]])
                    for t in range(2):
                        P.mm(psq, ONES256[:], YSQ[:, t, :], start=(t == 0), stop=(t == 1), w=[banks[1]])
                    P.cp(A, MEAN[:], pmean, r=[banks[0]])
                    P.act(VAR[:], pmean, AF.Square, r=[banks[0]])
                    yield
                    P.tt(V, VAR[:], psq, VAR[:], ALU.subtract, r=[banks[1], VAR])
                    P.act(RSTD[:], VAR[:], AF.Sqrt, bias=1e-5)
                    P.op(V, lambda e: e.reciprocal(out=RSTD[:], in_=RSTD[:]), [RSTD], [RSTD])
                    yield
                    for t in range(2):
                        P.tt(V, CT[:], YB[:, t, :], MEAN[:], ALU.subtract)
                        P.tt(V, CT[:], CT[:], RSTD[:], ALU.mult)
                        P.act(YMIX[:, 2 + t, :], CT[:], AF.Silu, bias=PV[:, LNB + t:LNB + t + 1], scale=PV[:, LNG + t:LNG + t + 1],
                              w=[(ymn, 2 + t)])
                        yield
                for q in range(14):
                    if lite and (q < 4 or q == 13):
                        continue
                    pb = q % 2
                    pm = proj(6 + q, pb)
                    mu = PV[:, MU + q:MU + q + 1]
                    omu = PV[:, OMU + q:OMU + q + 1]
                    P.act(TMPM[:, 1:TT], pm[:, 0:TT - 1], AF.Identity, scale=mu, r=[banks[pb], PV], w=[TMPM])
                    P.ts(V, TMPM[:, 0:1], HALO[:, q:q + 1], mu, None, ALU.mult, w=[TMPM])
                    P.cp(A, HALO[:, q:q + 1], pm[:, TT - 1:TT], r=[banks[pb]], w=[HALO])
                    if q < 12:
                        P.stt(CF[:, q, :], pm, omu, TMPM[:], ALU.mult, ALU.add, r=[banks[pb], PV, TMPM], w=[("CF", q)])
                    else:
                        P.stt(CT[:], pm, omu, TMPM[:], ALU.mult, ALU.add, r=[banks[pb], PV, TMPM], w=[CT])
                        if q == 12:
                            P.act(LACT[0:64, 0, :], CT[0:64, :], AF.Tanh, w=[LACT])
                            P.cp(V, LACT[64:128, 0, :], CT[64:128, :], w=[LACT])
                        else:
                            P.act(LACT[:, 1, :], CT[:], AF.Sigmoid, w=[LACT])
                    yield
                CFr = [("CF", q) for q in range(12)]
                if l == 0:
                    P.dma(SY, vfirst[:, t0:t0 + TT].rearrange("(c p) t -> p c t", p=128), CF[:, 8:12, :], reads=CFr, writes=["vfirst"])
                    for hp in range(4):
                        P.cp(G, VB[:, hp, :], CF[:, 8 + hp, :], r=CFr, w=[("VB", hp)])
                    yield
                else:
                    P.dma(SY, VFT[:], vfirst[:, t0:t0 + TT].rearrange("(c p) t -> p c t", p=128), reads=["vfirst"], writes=[VFT])
                    for hp in range(4):
                        P.cp(G, VB[:, hp, :], CF[:, 8 + hp, :], r=CFr, w=[("VB", hp)])
                    yield
                    pv1 = banks[0][0:32, 0:TT]
                    for hp in range(4):
                        P.mm(pv1, V1[:, hp, :], VB[:, hp, :], start=(hp == 0), stop=(hp == 3), r=[V1] + [("VB", h) for h in range(4)], w=[banks[0]])
                    P.cp(A, T1[:], pv1, r=[banks[0]])
                    yield
                    for hp in range(4):
                        pv2 = banks[1][:, 0:TT]
                        P.mm(pv2, V2[0:32, hp * 128:(hp + 1) * 128], T1[:], w=[banks[1]])
                        P.act(SG[:], pv2, AF.Sigmoid, bias=PV[:, V0c + hp:V0c + hp + 1], r=[banks[1], PV])
                        P.tt(V, CT[:], VFT[:, hp, :], CF[:, 8 + hp, :], ALU.subtract, r=[VFT] + CFr)
                        P.tt(V, CT[:], CT[:], SG[:], ALU.mult)
                        P.tt(V, CF[:, 8 + hp, :], CF[:, 8 + hp, :], CT[:], ALU.add, r=CFr + [CT], w=[("CF", 8 + hp)])
                        yield
                    for hp in range(4):
                        P.cp(G, VB[:, hp, :], CF[:, 8 + hp, :], r=CFr, w=[("VB", hp)])
                    yield
                for hp in range(4):
                    rr = CF[:, hp, :]
                    kk_ = CF[:, 4 + hp, :]
                    pw = banks[0][:, 0:TT]
                    pa = banks[1][:, 0:TT]
                    fs = slice(hp * 128, (hp + 1) * 128)
                    P.mm(pw, W2A2[0:64, fs], LACT[0:64, 0, :], w=[banks[0]])
                    P.mm(pa, W2A2[64:128, fs], LACT[64:128, 0, :], w=[banks[1]])
                    P.act(SGW[:], pw, AF.Sigmoid, bias=PV[:, W0 + hp:W0 + hp + 1], r=[banks[0], PV])
                    P.act(AA[:], pa, AF.Sigmoid, bias=PV[:, A0 + hp:A0 + hp + 1], r=[banks[1], PV])
                    yield
                    P.op(V, lambda e: e.tensor_tensor_scan(out=CUM[:], data0=CMASK[:], data1=SGW[:], initial=0.0, op0=ALU.mult, op1=ALU.add),
                         [CMASK, SGW], [CUM])
                    P.tt(G, CUMP[:], CUM[:], SGW[:], ALU.subtract)
                    P.act(ER[:, hp, :], CUM[:], AF.Exp, scale=-CDEC, w=[("ER", hp)])
                    P.act(EA[:], CUMP[:], AF.Exp, scale=-CDEC)
                    P.act(EI[:], CUM[:], AF.Exp, scale=CDEC)
                    yield
                    ER3 = ER[:, hp, :].rearrange("p (c t) -> p c t", t=64)
                    P.tt(V, EH[:].rearrange("p (c t) -> p c t", t=64), EI[:].rearrange("p (c t) -> p c t", t=64),
                         ER3[:, :, 63:64].to_broadcast([128, NCH, 64]), ALU.mult, r=[EI, ("ER", hp)], w=[EH])
                    P.ts(V, KK[:], kk_, PV[:, KKc + hp:KKc + hp + 1], None, ALU.mult, r=CFr + [PV])
                    P.act(KSQ[:], KK[:], AF.Square)
                    pss = banks[0][:, 0:TT]
                    P.mm(pss, BLK[:], KSQ[:], w=[banks[0]])
                    P.act(RN[:], pss, AF.Sqrt, bias=1e-24, r=[banks[0]])
                    yield
                    P.op(V, lambda e: e.reciprocal(out=RN[:], in_=RN[:]), [RN], [RN])
                    P.tt(V, KK[:], KK[:], RN[:], ALU.mult)
                    P.ts(V, TM2[:], AA[:], PV[:, KAc + hp:KAc + hp + 1], PV[:, OMKA + hp:OMKA + hp + 1], ALU.mult, ALU.add)
                    P.tt(V, KF[:], kk_, TM2[:], ALU.mult, r=CFr + [TM2])
                    P.tt(G, BETA[:], AA[:], KK[:], ALU.mult)
                    yield
                    AR3 = AR[:, hp, :, :]
                    P.stt(AR3[:, :, 0:64], KK[:].rearrange("p (c t) -> p c t", t=64), -1.0, EA[:].rearrange("p (c t) -> p c t", t=64),
                          ALU.mult, ALU.mult, r=[KK, EA], w=[(arn, hp)])
                    if not lite:
                        P.tt(V, AR3[:, :, 64:128], rr.rearrange("p (c t) -> p c t", t=64), ER3, ALU.mult, r=CFr + [("ER", hp)], w=[(arn, hp)])
                    P.tt(V, BT[:, hp, :], BETA[:], EI[:], ALU.mult, w=[(btn, hp)])
                    P.tt(G, KT[:, hp, :], KF[:], EI[:], ALU.mult, w=[(ktn, hp)])
                    yield
                    P.tt(V, BH[:, hp, :], BETA[:], EH[:], ALU.mult, w=[("BH", hp)])
                    P.tt(G, KH[:, hp, :], KF[:], EH[:], ALU.mult, w=[("KH", hp)])
                    if not lite:
                        P.stt(RKB[:, hp, :], rr, PV[:, RKc + hp:RKc + hp + 1], KF[:], ALU.mult, ALU.mult, r=CFr + [PV, KF], w=[RKB])
                    yield
                ARk = [(arn, h) for h in range(4)]
                BTk = [(btn, h) for h in range(4)]
                KTk = [(ktn, h) for h in range(4)]
                P.dma(SY, ARo[:], AR[64:128, :, :, :], reads=ARk, writes=[ARo])
                P.dma(SY, BTo[:], BT[64:128, :, :], reads=BTk, writes=[BTo])
                P.dma(SY, KTo[:], KT[64:128, :, :], reads=KTk, writes=[KTo])
                ER4 = ER[:].rearrange("p h (c t) -> p h c t", t=64)
                P.cp(V, ECe[:], ER4[:, :, :, 63], r=[("ER", h) for h in range(4)], w=[ECe])
                P.dma(SY, ECo[:], ECe[64:128, :, :], reads=[ECe], writes=[ECo])
                yield
                for c in range(NCH):
                    cs = slice(c * 64, (c + 1) * 64)
                    for (srcT, dstT, kn) in ((VB, VTOK, "VB"), (BH, BHT, "BH"), (KH, KHT, "KH")):
                        pT = bview(7, BF16, "p (a b) -> p a b", b=128)
                        for hp in range(4):
                            P.tr(pT[0:64, hp, :], srcT[:, hp, cs], identb[:], r=[(kn, hp), identb], w=[banks[7]])
                        P.cp(A, dstT[:, c, :].rearrange("p (a b) -> p a b", b=128), pT[0:64, 0:4, :], r=[banks[7]], w=[dstT])
                        yield

            def scan(it):
                b = it % 2
                t0 = it * TT
                lite = it < first_out_tile - 1
                HX, LACT, AR, BT, KT, RKB = HXs[b], LACTs[b], ARs[b], BTs[b], KTs[b], RKBs[b]
                ARo, BTo, KTo, ECo, ECe = ARos[b], BTos[b], KTos[b], ECos[b], ECes[b]
                VTOK, BHT, KHT, YMIX = VTOKs[b], BHTs[b], KHTs[b], YMIXs[b]
                ymn = "YMIX%d" % b
                arn, btn, ktn = "AR%d" % b, "BT%d" % b, "KT%d" % b
                ARk = [(arn, h) for h in range(4)]
                BTk = [(btn, h) for h in range(4)]
                KTk = [(ktn, h) for h in range(4)]

                def fm(even, odd, h):
                    return (even[0:64, h // 2] if h % 2 == 0 else odd[0:64, h // 2])
                mar = MASKAR[:].unsqueeze(1).to_broadcast([64, 4, 128])
                for c in range(NCH):
                    cs = slice(c * 64, (c + 1) * 64)
                    rdk = ARk + BTk + KTk + [ARo, BTo, KTo]
                    pMa = bview(2, F32, "p (h x) -> p h x", x=128)
                    pMb = bview(3, F32, "p (h x) -> p h x", x=128)

                    def pM(h):
                        return (pMa if h < 4 else pMb)[0:64, h % 4, :]
                    pMk = [banks[2], banks[3]]
                    for h in range(8):
                        P.mm(pM(h), fm(BT, BTo, h)[:, cs], fm(AR, ARo, h)[:, c, :], r=rdk, w=[pMk[h // 4]])
                    P.tt(V, MBs[:, 0:4, :], pMa[0:64], mar, ALU.mult, r=[banks[2], MASKAR], w=[MBs])
                    P.tt(V, MBs[:, 4:8, :], pMb[0:64], mar, ALU.mult, r=[banks[3], MASKAR], w=[MBs])
                    yield
                    for h in range(8):
                        P.mm(pM(h), fm(KT, KTo, h)[:, cs], fm(AR, ARo, h)[:, c, :], r=rdk, w=[pMk[h // 4]])
                    P.tt(V, MKs[:, 0:4, :], pMa[0:64], mar, ALU.mult, r=[banks[2], MASKAR], w=[MKs])
                    P.tt(V, MKs[:, 4:8, :], pMb[0:64], mar, ALU.mult, r=[banks[3], MASKAR], w=[MKs])
                    yield
                    pC = bview(4, F32, "p (h x) -> p h x", x=64)
                    pC2 = bview(2, F32, "p (h x) -> p h x", x=64)
                    pC3 = bview(3, F32, "p (h x) -> p h x", x=64)
                    for h in range(8):
                        P.mm(pC[0:64, h, :], fm(AR, ARo, h)[:, c, 0:64], fm(BT, BTo, h)[:, cs], r=rdk, w=[banks[4]])
                    P.tt(V, Np[0][:], pC[0:64], MASKLOW[:].unsqueeze(1).to_broadcast([64, 8, 64]), ALU.mult, r=[banks[4], MASKLOW])
                    P.cp(G, Mp[0][:], MBs[:, :, 0:64])
                    P.tt(G, Tb[:], MBs[:, :, 0:64], I8[:].unsqueeze(1).to_broadcast([64, 8, 64]), ALU.add, r=[MBs, I8])
                    yield
                    cur = 0
                    for step in range(5):
                        nxt = 1 - cur
                        last = (step == 4)
                        for h in range(8):
                            P.mm(pC[0:64, h, :], Mp[cur][:, h, :], Np[cur][:, h, :], w=[banks[4]])
                        P.cp(A, Np[nxt][:], pC[0:64], r=[banks[4]])
                        if not last:
                            for h in range(8):
                                P.mm(pC2[0:64, h, :], Np[cur][:, h, :], Mp[cur][:, h, :], w=[banks[2]])
                            P.cp(V, Mp[nxt][:], pC2[0:64], r=[banks[2]])
                        yield
                        for h in range(8):
                            P.mm(pC3[0:64, h, :], Np[nxt][:, h, :], Tb[:, h, :], w=[banks[3]])
                        P.tt(V, Tb[:], Tb[:], pC3[0:64], ALU.add, r=[Tb, banks[3]])
                        cur = nxt
                        yield
                    pZ = bview(5, F32, "p (h x) -> p h x", x=64)
                    for h in range(8):
                        hs = slice(h * 64, (h + 1) * 64)
                        P.mm(pZ[0:64, h, :], fm(AR, ARo, h)[:, c, 0:64], Sb[:, h, :], start=True, stop=False, r=rdk + [Sb], w=[banks[5]])
                        P.mm(pZ[0:64, h, :], MKs[:, h, 0:64], VTOK[:, c, hs], start=False, stop=True, w=[banks[5]])
                    P.cp(A, WTs[:], pZ[0:64], r=[banks[5]])
                    yield
                    for h in range(8):
                        P.mm(pZ[0:64, h, :], Tb[:, h, :], WTs[:, h, :], w=[banks[5]])
                    P.cp(V, UTs[:], pZ[0:64], r=[banks[5]])
                    yield
                    if not lite:
                        pY = bview(6, F32, "p (h x) -> p h x", x=64)
                        for h in range(8):
                            hs = slice(h * 64, (h + 1) * 64)
                            P.mm(pY[0:64, h, :], fm(AR, ARo, h)[:, c, 64:128], Sb[:, h, :], start=True, stop=False, r=rdk + [Sb], w=[banks[6]])
                            P.mm(pY[0:64, h, :], MBs[:, h, 64:128], UTs[:, h, :], start=False, stop=False, w=[banks[6]])
                            P.mm(pY[0:64, h, :], MKs[:, h, 64:128], VTOK[:, c, hs], start=False, stop=True, w=[banks[6]])
                        yield
                    for h in range(8):
                        hs = slice(h * 64, (h + 1) * 64)
                        P.mm(pZ[0:64, h, :], BHT[:, c, hs], UTs[:, h, :], start=True, stop=False, w=[banks[5]])
                        P.mm(pZ[0:64, h, :], KHT[:, c, hs], VTOK[:, c, hs], start=False, stop=True, w=[banks[5]])
                    for par, ecsrc in ((0, ECe[0:64, :, c:c + 1]), (1, ECo[:, :, c:c + 1])):
                        Sv = Sf[:].rearrange("p (h two) x -> p h two x", two=2)[:, :, par, :]
                        P.tt(V, Sv, Sv, ecsrc.to_broadcast([64, 4, 64]), ALU.mult, r=[Sf, ECo, ECe], w=[Sf])
                    P.tt(V, Sf[:], Sf[:], pZ[0:64], ALU.add, r=[Sf, banks[5]])
                    P.cp(A, Sb[:], Sf[:])
                    yield
                    if lite:
                        continue
                    P.cp(A, YS[:], pY[0:64], r=[banks[6]])
                    P.act(YQ[:], pY[0:64], AF.Square, r=[banks[6]])
                    P.op(V, lambda e: e.tensor_reduce(out=ST[:, 0:8], in_=YS[:], axis=AX.X, op=ALU.add), [YS], [ST])
                    P.op(V, lambda e: e.tensor_reduce(out=ST[:, 8:16], in_=YQ[:], axis=AX.X, op=ALU.add), [YQ], [ST])
                    yield
                    P.ts(V, ST[:, 16:24], ST[:, 0:8], 1.0 / 64, None, ALU.mult)
                    P.tt(V, ST[:, 24:32], ST[:, 16:24], ST[:, 16:24], ALU.mult)
                    P.stt(ST[:, 32:40], ST[:, 8:16], 1.0 / 64, ST[:, 24:32], ALU.mult, ALU.subtract)
                    P.act(ST[:, 32:40], ST[:, 32:40], AF.Sqrt, bias=64e-5)
                    P.op(V, lambda e: e.reciprocal(out=ST[:, 32:40], in_=ST[:, 32:40]), [ST], [ST])
                    yield
                    P.tt(V, YS[:], YS[:], ST[:, 16:24].unsqueeze(2).to_broadcast([64, 8, 64]), ALU.subtract, r=[YS, ST])
                    P.tt(V, YS[:], YS[:], ST[:, 32:40].unsqueeze(2).to_broadcast([64, 8, 64]), ALU.mult, r=[YS, ST])
                    YS2 = YS[:].rearrange("p h x -> p (h x)")
                    P.tt(V, YS2, YS2, GNG[:], ALU.mult, r=[YS, GNG], w=[YS])
                    P.tt(G, YS2, YS2, GNB[:], ALU.add, r=[YS, GNB], w=[YS])
                    yield
                    pBn = banks[3][0:64, 0:8]
                    for hp in range(4):
                        P.mm(pBn, RKB[:, hp, cs], HSEL[:, hp, :], start=(hp == 0), stop=(hp == 3), w=[banks[3]])
                    P.cp(A, BON[:], pBn, r=[banks[3]])
                    P.tt(V, YQ[:], VTOK[:, c, :].rearrange("p (h x) -> p h x", x=64), BON[:].unsqueeze(2).to_broadcast([64, 8, 64]), ALU.mult,
                         r=[VTOK, BON], w=[YQ])
                    P.tt(V, YS[:], YS[:], YQ[:], ALU.add)
                    yield
                    pG = banks[2][0:64, :]
                    P.mm(pG, LACT[:, 1, cs], G2[:], w=[banks[2]])
                    P.tt(V, YM[:], YS2, pG, ALU.mult, r=[YS, banks[2]], w=[YM])
                    pT2 = bview(4, BF16, "p (a b) -> p a b", b=128)
                    for hp in range(4):
                        P.tr(pT2[:, hp, 0:64], YM[:, hp * 128:(hp + 1) * 128], identb[0:64, 0:64], r=[YM, identb], w=[banks[4]])
                    P.cp(A, YMIX[:, 4:8, cs], pT2[:, 0:4, 0:64], r=[banks[4]], w=[(ymn, "rw")])
                    yield
                if it >= first_out_tile:
                    ymk = [(ymn, i) for i in (0, 1, 2, 3, "rw")]
                    for s in range(NSUB):
                        for dh in range(2):
                            po = banks[2 + dh][:, :]
                            for fc in range(8):
                                P.mm(po, YMIX[:, fc, s * 128:(s + 1) * 128], WOUT[:, fc, dh * 512:(dh + 1) * 512],
                                     start=(fc == 0), stop=(fc == 7), r=ymk + [WOUT], w=[banks[2 + dh]])
                            P.tt(V, HX[:, s, dh * 512:(dh + 1) * 512], HX[:, s, dh * 512:(dh + 1) * 512], po, ALU.add, r=[HX, banks[2 + dh]], w=[HX])
                            yield
                    d0 = t0 - first_out_tile * TT
                    P.dma(SY, dst[d0:d0 + TT, :].rearrange("(s p) d -> p s d", p=128), HX[:], reads=[HX], writes=["dst%d" % l])
                yield

            for it in range(NTILE + 1):
                gens = []
                if it < NTILE:
                    gens.append(prep(it))
                if it >= 1:
                    gens.append(scan(it - 1))
                while gens:
                    for g_ in list(gens):
                        try:
                            next(g_)
                        except StopIteration:
                            gens.remove(g_)
                chk("m0T%d" % it)

    def ffn(l, src, src_row0, ntok, dst, dst_is_final):
        moe = (l == 1)
        E = NE if moe else 1
        F = DFE if moe else DFF
        FG = 256
        NFG = F // FG
        NJ = GT // 128
        TW = min(512, GT)
        NTT = GT // TW
        NSW = TW // 128
        with P.scope():
            G2n = P.sb([128, D], F32, "G2n")
            P.dma(SY, G2n[:], norm2_g[l].partition_broadcast(128))
            if dst_is_final:
                GF = P.sb([128, D], F32, "GF")
                P.dma(SY, GF[:], final_g.partition_broadcast(128))
            if moe:
                RT = P.sb([128, 8, NE], F32, "RT")
                P.dma(SY, RT[:], moe_router[0].rearrange("(c p) e -> p c e", p=128))
                XNF = P.sb([128, D], F32, "XNF")
                XNTF = P.sb([128, 8, 128], F32, "XNTF")
                LOG = P.sb([128, 8], F32, "LOG")
                MX = P.sb([128, 8], F32, "MX")
                G12 = P.sb([128, 2], F32, "G12")
                EQ = P.sb([128, 8], F32, "EQ")
                GATE = P.sb([128, NJ, NE], F32, "GATE")
            ACC = P.sb([128, NJ, D], F32, "ACC")
            HNT = P.sb([128, 8, GT], BF16, "HNT2")
            SQ = P.sb([128, D], F32, "SQ2")
            SS = P.sb([128, NJ], F32, "SS2")
            RS = P.sb([128, NJ], F32, "RS2")
            XN = P.sb([128, D], BF16, "XN2")
            WG = [P.sb([128, 8, FG], BF16, f"WG{i}") for i in range(2)]
            WU = [P.sb([128, 8, FG], BF16, f"WU{i}") for i in range(2)]
            WD = [P.sb([128, 2, D], BF16, f"WD{i}") for i in range(2)]
            SIL = [P.sb([128, TW], F32, f"SIL{i}") for i in range(2)]
            ACTT = [P.sb([128, 2, TW], BF16, f"ACTT{i}") for i in range(2)]
            for g in range(ntok // GT):
                r0 = src_row0 + g * GT
                for j in range(NJ):
                    P.dma(SY, ACC[:, j, :], src[r0 + j * 128:r0 + (j + 1) * 128, :], reads=["src%d" % l], writes=[("ACC", j)])
                for j in range(NJ):
                    ak = ("ACC", j)
                    P.act(SQ[:], ACC[:, j, :], AF.Square, accum_out=SS[:, j:j + 1], r=[ak], w=[SQ, SS])
                    P.act(RS[:, j:j + 1], SS[:, j:j + 1], AF.Sqrt, bias=1e-6, scale=1.0 / D)
                    P.op(V, lambda e, j=j: e.reciprocal(out=RS[:, j:j + 1], in_=RS[:, j:j + 1]), [RS], [RS])
                    P.stt(XN[:], ACC[:, j, :], RS[:, j:j + 1], G2n[:], ALU.mult, ALU.mult, r=[ak, RS, G2n])
                    pT = bview(7, BF16, "p (a b) -> p a b", b=128)
                    for c in range(8):
                        P.tr(pT[:, c, :], XN[:, c * 128:(c + 1) * 128], identb[:], w=[banks[7]])
                    P.cp(V, HNT[:, :, j * 128:(j + 1) * 128], pT, r=[banks[7]], w=[("HNT2", j // NSW)])
                    if moe:
                        P.stt(XNF[:], ACC[:, j, :], RS[:, j:j + 1], G2n[:], ALU.mult, ALU.mult, r=[ak, RS, G2n])
                        for half in range(2):
                            pTf = bview(5 + half, F32, "p (a b) -> p a b", b=128)
                            for c4 in range(4):
                                c = half * 4 + c4
                                P.tr(pTf[:, c4, :], XNF[:, c * 128:(c + 1) * 128], identf[:], w=[banks[5 + half]])
                            P.cp(A, XNTF[:, half * 4:half * 4 + 4, :], pTf, r=[banks[5 + half]], w=[XNTF])
                        pR = banks[4][:, 0:NE]
                        for c in range(8):
                            P.mm(pR, XNTF[:, c, :], RT[:, c, :], start=(c == 0), stop=(c == 7), w=[banks[4]])
                        P.cp(A, LOG[:], pR, r=[banks[4]])
                        P.op(V, lambda e: e.max(out=MX[:], in_=LOG[:]), [LOG], [MX])
                        P.tt(V, G12[:, 0:1], MX[:, 0:1], MX[:, 1:2], ALU.subtract, w=[G12])
                        P.act(G12[:, 1:2], G12[:, 0:1], AF.Sigmoid, scale=-1.0)
                        P.act(G12[:, 0:1], G12[:, 0:1], AF.Sigmoid)
                        P.ts(V, EQ[:], LOG[:], MX[:, 0:1], G12[:, 0:1], ALU.is_equal, ALU.mult)
                        P.ts(V, GATE[:, j, :], LOG[:], MX[:, 1:2], G12[:, 1:2], ALU.is_equal, ALU.mult, w=[GATE])
                        P.tt(V, GATE[:, j, :], GATE[:, j, :], EQ[:], ALU.add, r=[GATE, EQ], w=[GATE])
                widx = 0
                for e in range(E):
                    wg_d = (moe_w_gate[0, e] if moe else ffn_w_gate[0])
                    wu_d = (moe_w_up[0, e] if moe else ffn_w_up[0])
                    wd_d = (moe_w_down[0, e] if moe else ffn_w_down[0])
                    for fg in range(NFG):
                        wb = widx % 2
                        widx += 1
                        f0 = fg * FG
                        P.dma(G, WG[wb][:], wg_d[:, f0:f0 + FG].rearrange("(c p) f -> p c f", p=128))
                        P.dma(G, WU[wb][:], wu_d[:, f0:f0 + FG].rearrange("(c p) f -> p c f", p=128))
                        P.dma(G, WD[wb][:], wd_d[f0:f0 + FG, :].rearrange("(c p) d -> p c d", p=128))
                        for tt_ in range(NTT):
                            ab = tt_ % 2
                            for fc in range(2):
                                pG_ = banks[0 + 2 * fc][:, 0:TW]
                                pU_ = banks[1 + 2 * fc][:, 0:TW]
                                for c in range(8):
                                    P.mm(pG_, WG[wb][:, c, fc * 128:(fc + 1) * 128], HNT[:, c, tt_ * TW:(tt_ + 1) * TW],
                                         start=(c == 0), stop=(c == 7), r=[WG[wb], ("HNT2", tt_)], w=[banks[0 + 2 * fc]])
                                for c in range(8):
                                    P.mm(pU_, WU[wb][:, c, fc * 128:(fc + 1) * 128], HNT[:, c, tt_ * TW:(tt_ + 1) * TW],
                                         start=(c == 0), stop=(c == 7), r=[WU[wb], ("HNT2", tt_)], w=[banks[1 + 2 * fc]])
                                P.act(SIL[fc][:], pG_, AF.Silu, r=[banks[0 + 2 * fc]])
                                P.tt(V, ACTT[ab][:, fc, :], SIL[fc][:], pU_, ALU.mult, r=[SIL[fc], banks[1 + 2 * fc]], w=[("ACTT%d" % ab, fc)])
                            for s in range(NSW):
                                j = tt_ * NSW + s
                                for dh in range(2):
                                    pb = 4 + (s % 2) * 2 + dh
                                    pD = banks[pb][:, :]
                                    for fc in range(2):
                                        P.mm(pD, ACTT[ab][:, fc, s * 128:(s + 1) * 128], WD[wb][:, fc, dh * 512:(dh + 1) * 512],
                                             start=(fc == 0), stop=(fc == 1), r=[("ACTT%d" % ab, 0), ("ACTT%d" % ab, 1), WD[wb]], w=[banks[pb]])
                                    accv = ACC[:, j, dh * 512:(dh + 1) * 512]
                                    if moe:
                                        P.stt(accv, pD, GATE[:, j, e:e + 1], accv, ALU.mult, ALU.add, r=[banks[pb], GATE, ("ACC", j)], w=[("ACC", j)])
                                    else:
                                        P.tt(V, accv, accv, pD, ALU.add, r=[banks[pb], ("ACC", j)], w=[("ACC", j)])
                for j in range(NJ):
                    ak = ("ACC", j)
                    row = g * GT + j * 128
                    if dst_is_final:
                        P.act(SQ[:], ACC[:, j, :], AF.Square, accum_out=SS[:, j:j + 1], r=[ak], w=[SQ, SS])
                        P.act(RS[:, j:j + 1], SS[:, j:j + 1], AF.Sqrt, bias=1e-6, scale=1.0 / D)
                        P.op(V, lambda e, j=j: e.reciprocal(out=RS[:, j:j + 1], in_=RS[:, j:j + 1]), [RS], [RS])
                        P.stt(ACC[:, j, :], ACC[:, j, :], RS[:, j:j + 1], GF[:], ALU.mult, ALU.mult, r=[ak, RS, GF], w=[ak])
                        out_ops.append(P.dma(SY, dst[row:row + 128, :], ACC[:, j, :], reads=[ak], writes=["out"]))
                    else:
                        jj = (src_row0 + row) // 128
                        P.ts(V, ACC[:, j, :], ACC[:, j, :], padmask[:, jj:jj + 1], None, ALU.mult, r=[ak, padmask], w=[ak])
                        P.dma(SY, dst[src_row0 + row:src_row0 + row + 128, :], ACC[:, j, :], reads=[ak], writes=["dst_ffn%d" % l])

    try:
        mixer(0, xs, hA, 0)
        P.barrier()
        chk("m0")
        ffn(0, hA, 0, NTOK, hB, False)
        P.barrier()
        chk("f0")
        mixer(1, hB, hA, NTILE // 2)
        P.barrier()
        chk("m1")
        ffn(1, hA, 0, NMOE, out_d, True)
    except _Stop:
        pass
    P.emit((SY, out_ops))
    return nc


def make_consts(TT=128):
    i = np.arange(64)[:, None]
    t = np.arange(64)[None, :]
    strict = (i < t).astype(np.float32)
    incl = (i <= t).astype(np.float32)
    maskar = np.concatenate([strict, incl], 1)
    masklow = (i > t).astype(np.float32)
    i8 = np.eye(64, dtype=np.float32)
    cmask = np.ones((128, TT), np.float32)
    cmask[:, ::64] = 0.0
    p = np.arange(128)
    blockones = (p[:, None] // 64 == p[None, :] // 64).astype(np.float32)
    headsel = np.zeros((128, 4, 8), np.float32)
    for hp in range(4):
        headsel[p, hp, 2 * hp + p // 64] = 1.0
    return {"c_ident": np.eye(128, dtype=np.float32), "c_maskar": np.ascontiguousarray(maskar),
            "c_masklow": np.ascontiguousarray(masklow), "c_i8": np.ascontiguousarray(i8), "c_cmask": cmask,
            "c_blockones": blockones, "c_headsel": headsel}


def make_core_inputs(x, NTOK, s):
    half = NTOK // 2
    wins = [2.0, 4.0, 8.0, 16.0]
    fixv = np.ones((128, 2, 16), np.float32)
    pos = np.arange(16) + 1.0
    for t in range(2):
        for hh in range(2):
            w = wins[2 * t + hh]
            fixv[hh * 64:(hh + 1) * 64, t, :] = w / np.minimum(pos, w)
    pfix = np.ones((128, 2, 2, 16), np.float32)
    if s == 1:
        xs = np.ascontiguousarray(x[:NTOK])
        mask = np.ones(NTOK, np.float32)
        pfix[:, :, 0, :] = fixv
    else:
        xs = np.concatenate([np.zeros((half, x.shape[1]), np.float32), x[:half]], 0)
        mask = np.concatenate([np.zeros(half, np.float32), np.ones(half, np.float32)])
        pfix[:, :, 1, :] = fixv
    padmask = np.ascontiguousarray(mask.reshape(NTOK // 128, 128).T)
    return {"xs": xs, "padmask": padmask, "pfix": pfix}


_WNAMES = ["norm1_g", "w_in", "pool_w", "pool_scale", "conv_w", "conv_b", "conv_ln_g", "conv_ln_b", "shift_mu",
           "rwkv_w0", "rwkv_w2", "rwkv_a0", "rwkv_a2", "rwkv_g2", "rwkv_k_k", "rwkv_k_a", "rwkv_r_k", "rwkv_gn_g",
           "rwkv_gn_b", "rwkv_v0", "rwkv_v1", "rwkv_v2", "w_out", "norm2_g", "ffn_w_gate", "ffn_w_up", "ffn_w_down",
           "moe_router", "moe_w_gate", "moe_w_up", "moe_w_down", "final_g"]


def run(inputs, NTOK=SEQ, GT=2048, TT=128):
    x = np.asarray(inputs["x"], np.float32)
    B = x.shape[0]
    nc = build(NTOK, GT, TT)
    consts = make_consts(TT)
    wts = {}
    for n in _WNAMES:
        a = np.ascontiguousarray(np.asarray(inputs[n], np.float32))
        if n == "rwkv_r_k":
            a = a.reshape(2, 512)
        if _KSTOP is not None and n in ("moe_w_gate", "moe_w_up"):
            a = np.ascontiguousarray(a[..., :8])
        if _KSTOP is not None and n == "moe_w_down":
            a = np.ascontiguousarray(a[:, :, :8, :])
        wts[n] = a
    in_maps = []
    for c in range(2 * B):
        b, s = c // 2, c % 2
        m = dict(wts)
        m.update(consts)
        m.update(make_core_inputs(x[b], NTOK, s))
        in_maps.append(m)
    res = run_bass_kernel_spmd(nc, in_maps, core_ids=list(range(2 * B)))
    half = NTOK // 2
    out = np.zeros((B, NTOK, D), np.float32)
    for c in range(2 * B):
        b, s = c // 2, c % 2
        out[b, s * half:(s + 1) * half] = res.results[c]["out"]
    return out


def kernel(**inputs):
    return run(inputs, SEQ, 2048, 128)
```

```python
from contextlib import ExitStack
import numpy as np
import concourse.bass as bass
import concourse.mybir as mybir
from concourse.bass_utils import run_bass_kernel_spmd

F32 = mybir.dt.float32
BF16 = mybir.dt.bfloat16
AF = mybir.ActivationFunctionType
ALU = mybir.AluOpType
AX = mybir.AxisListType

D = 1024
NIN = 2560
DFF = 2816
DFE = 3584
NE = 8
SEQ = 8192
NDMA_SLOTS = 8
_KSTOP = None
_KSKIP = ()


class _Stop(Exception):
    pass


def chk(tag):
    if _KSTOP == tag:
        raise _Stop()
V, A, G, T, SY = "vector", "scalar", "gpsimd", "tensor", "sync"
CDEC = 0.6065306597126334


class Prog:
    def __init__(self, nc):
        self.nc = nc
        self.ops = []
        self.last_write = {}
        self.readers = {}
        self.stacks = [ExitStack()]
        self.ntile = 0
        self.dnext = {SY: 0, A: 0, G: 0}
        self.last_on = {}
        self.bar = set()
        self.base = {}

    def scope(self):
        prog = self

        class _S:
            def __enter__(s):
                prog.stacks.append(ExitStack())

            def __exit__(s, *a):
                prog.barrier()
                prog.stacks.pop().close()
                return False
        return _S()

    def sb(self, shape, dtype, name):
        self.ntile += 1
        self.base[f"{name}_{self.ntile}"] = name
        return self.stacks[-1].enter_context(self.nc.sbuf_tensor(f"{name}_{self.ntile}", list(shape), dtype))

    def ps(self, shape, dtype, name):
        self.ntile += 1
        self.base[f"{name}_{self.ntile}"] = name
        return self.stacks[-1].enter_context(self.nc.psum_tensor(f"{name}_{self.ntile}", list(shape), dtype))

    def _k(self, x):
        if isinstance(x, (str, tuple)):
            return x
        return self.base.get(x.name, x.name)

    def barrier(self):
        self.bar = set(self.last_on.values())

    def op(self, eng, fn, reads=(), writes=(), dma=False):
        i = len(self.ops)
        deps = set(self.bar)
        reads = [self._k(x) for x in reads]
        writes = [self._k(x) for x in writes]
        if dma:
            slot = self.dnext[eng]
            self.dnext[eng] = (slot + 1) % NDMA_SLOTS
            semkey = (eng, slot)
        else:
            semkey = eng
        for k in reads:
            j = self.last_write.get(k)
            if j is not None:
                deps.add(j)
            if isinstance(k, str) and k.startswith("bank"):
                deps.update(v for sk, v in self.readers.get(k, {}).items() if sk != semkey)
        for k in writes:
            j = self.last_write.get(k)
            if j is not None:
                deps.add(j)
            deps.update(self.readers.get(k, {}).values())
        for k in reads:
            self.readers.setdefault(k, {})[semkey] = i
        for k in writes:
            self.last_write[k] = i
            self.readers[k] = {}
        self.last_on[semkey] = i
        self.ops.append((eng, fn, deps, semkey))
        return i

    def dma(self, q, out, in_, reads=None, writes=None, **kw):
        return self.op(q, lambda e: e.dma_start(out=out, in_=in_, **kw),
                       [in_] if reads is None else reads, [out] if writes is None else writes, dma=True)

    def tt(self, eng, out, in0, in1, op, r=None, w=None):
        return self.op(eng, lambda e: e.tensor_tensor(out=out, in0=in0, in1=in1, op=op),
                       [in0, in1] if r is None else r, [out] if w is None else w)

    def ts(self, eng, out, in0, s1, s2=None, op0=ALU.mult, op1=None, r=None, w=None):
        rr = [in0] + [x for x in (s1, s2) if not isinstance(x, (int, float, type(None)))]
        kw = {} if op1 is None else {"op1": op1}
        return self.op(eng, lambda e: e.tensor_scalar(out=out, in0=in0, scalar1=s1, scalar2=s2, op0=op0, **kw),
                       rr if r is None else r, [out] if w is None else w)

    def stt(self, out, in0, scalar, in1, op0, op1, r=None, w=None):
        rr = [in0, in1] + ([] if isinstance(scalar, (int, float)) else [scalar])
        return self.op(V, lambda e: e.scalar_tensor_tensor(out=out, in0=in0, scalar=scalar, in1=in1, op0=op0, op1=op1),
                       rr if r is None else r, [out] if w is None else w)

    def act(self, out, in_, func, bias=None, scale=1.0, accum_out=None, r=None, w=None):
        rr = [in_] + [x for x in (bias, scale) if not isinstance(x, (int, float, type(None)))]
        ww = [out] + ([accum_out] if accum_out is not None else [])
        kw = {}
        if bias is not None:
            kw["bias"] = bias
        if accum_out is not None:
            kw["accum_out"] = accum_out
        return self.op(A, lambda e: e.activation(out=out, in_=in_, func=func, scale=scale, **kw),
                       rr if r is None else r, ww if w is None else w)

    def cp(self, eng, out, in_, r=None, w=None):
        if eng == A:
            return self.act(out, in_, AF.Copy, r=r, w=w)
        return self.op(eng, lambda e: e.tensor_copy(out=out, in_=in_), [in_] if r is None else r, [out] if w is None else w)

    def mm(self, out, lhsT, rhs, start=True, stop=True, r=None, w=None):
        return self.op(T, lambda e: e.matmul(out, lhsT=lhsT, rhs=rhs, start=start, stop=stop),
                       [lhsT, rhs] if r is None else r, [out] if w is None else w)

    def tr(self, out, in_, ident, r=None, w=None):
        return self.op(T, lambda e: e.transpose(out=out, in_=in_, identity=ident),
                       [in_, ident] if r is None else r, [out] if w is None else w)

    def memset(self, eng, ap, val):
        return self.op(eng, lambda e: e.memset(ap, val), [], [ap])

    def emit(self, final_wait_ops):
        nc = self.nc
        engs = [SY, A, V, G, T]
        with ExitStack() as st:
            csem = {e: st.enter_context(nc.semaphore(f"c_{e}")) for e in engs if e != SY}
            dsem = {(e, i): st.enter_context(nc.semaphore(f"d_{e}{i}")) for e in (SY, A, G) for i in range(NDMA_SLOTS)}
            cnt = {}
            token = [None] * len(self.ops)
            prev = [None] * len(self.ops)
            per_eng = {e: [] for e in engs}
            for i, (eng, fn, deps, semkey) in enumerate(self.ops):
                c = cnt.get(semkey, 0)
                if isinstance(semkey, tuple):
                    prev[i] = (dsem[semkey], c * 16)
                    token[i] = (dsem[semkey], (c + 1) * 16)
                else:
                    token[i] = (csem[semkey], c + 1)
                cnt[semkey] = c + 1
                per_eng[eng].append(i)
            block = st.enter_context(nc.Block())
            ops = self.ops

            def body(engname):
                def run(e):
                    waited = {}
                    for i in per_eng[engname]:
                        eng, fn, deps, semkey = ops[i]
                        isdma = isinstance(semkey, tuple)
                        need = {}
                        for j in deps:
                            if engname == T and ops[j][3] == T:
                                continue
                            s, v = token[j]
                            if need.get(s.num, (None, 0))[1] < v:
                                need[s.num] = (s, v)
                        if isdma and prev[i][1] > 0:
                            s, v = prev[i]
                            if need.get(s.num, (None, 0))[1] < v:
                                need[s.num] = (s, v)
                        for s, v in need.values():
                            if waited.get(s.num, 0) < v:
                                e.wait_ge(s, v)
                                waited[s.num] = v
                        ins = fn(e)
                        ins.then_inc(token[i][0], 16 if isdma else 1)
                    if engname == final_wait_ops[0]:
                        for j in list(final_wait_ops[1]) + sorted(self.last_on.values()):
                            s, v = token[j]
                            if waited.get(s.num, 0) < v:
                                e.wait_ge(s, v)
                                waited[s.num] = v
                return run

            block.sync(body(SY))
            block.scalar(body(A))
            block.vector(body(V))
            block.gpsimd(body(G))
            block.tensor(body(T))
        self.stacks[0].close()


def build(NTOK=SEQ, GT=2048, TT=128):
    nc = bass.Bass("TRN2", target_bir_lowering=False)
    NMOE = NTOK // 2
    NCH = TT // 64
    NSUB = TT // 128
    NTILE = NTOK // TT

    def din(name, shape):
        return nc.dram_tensor(name, list(shape), F32, kind="ExternalInput").ap()

    xs = din("xs", [NTOK, D])
    padmask_d = din("padmask", [128, NTOK // 128])
    pfix_d = din("pfix", [128, 2, 2, 16])
    norm1_g = din("norm1_g", [2, D])
    w_in = din("w_in", [2, D, NIN])
    pool_w = din("pool_w", [2, 4, 64, 64])
    pool_scale = din("pool_scale", [2, 256])
    conv_w = din("conv_w", [2, 31, 256])
    conv_b = din("conv_b", [2, 256])
    conv_ln_g = din("conv_ln_g", [2, 256])
    conv_ln_b = din("conv_ln_b", [2, 256])
    shift_mu = din("shift_mu", [2, 1792])
    rwkv_w0 = din("rwkv_w0", [2, 512])
    rwkv_w2 = din("rwkv_w2", [2, 64, 512])
    rwkv_a0 = din("rwkv_a0", [2, 512])
    rwkv_a2 = din("rwkv_a2", [2, 64, 512])
    rwkv_g2 = din("rwkv_g2", [2, 128, 512])
    rwkv_k_k = din("rwkv_k_k", [2, 512])
    rwkv_k_a = din("rwkv_k_a", [2, 512])
    rwkv_r_k = din("rwkv_r_k", [2, 512])
    rwkv_gn_g = din("rwkv_gn_g", [2, 512])
    rwkv_gn_b = din("rwkv_gn_b", [2, 512])
    rwkv_v0 = din("rwkv_v0", [1, 512])
    rwkv_v1 = din("rwkv_v1", [1, 512, 32])
    rwkv_v2 = din("rwkv_v2", [1, 32, 512])
    w_out = din("w_out", [2, D, D])
    norm2_g = din("norm2_g", [2, D])
    ffn_w_gate = din("ffn_w_gate", [1, D, DFF])
    ffn_w_up = din("ffn_w_up", [1, D, DFF])
    ffn_w_down = din("ffn_w_down", [1, DFF, D])
    moe_router = din("moe_router", [1, D, NE])
    small = _KSTOP is not None
    moe_w_gate = din("moe_w_gate", [1, NE, D, 8 if small else DFE])
    moe_w_up = din("moe_w_up", [1, NE, D, 8 if small else DFE])
    moe_w_down = din("moe_w_down", [1, NE, 8 if small else DFE, D])
    final_g = din("final_g", [D])
    ident_d = din("c_ident", [128, 128])
    maskar_d = din("c_maskar", [64, 128])
    masklow_d = din("c_masklow", [64, 64])
    i8_d = din("c_i8", [64, 64])
    cmask_d = din("c_cmask", [128, TT])
    blockones_d = din("c_blockones", [128, 128])
    headsel_d = din("c_headsel", [128, 4, 8])
    out_d = nc.dram_tensor("out", [NMOE, D], F32, kind="ExternalOutput").ap()
    hA = nc.dram_tensor("hA", [NTOK, D], F32).ap()
    hB = nc.dram_tensor("hB", [NTOK, D], F32).ap()
    vfirst = nc.dram_tensor("vfirst", [512, NTOK], F32).ap()

    P = Prog(nc)
    out_ops = []

    identf = P.sb([128, 128], F32, "identf")
    identb = P.sb([128, 128], BF16, "identb")
    P.dma(SY, identf[:], ident_d)
    P.cp(V, identb[:], identf[:])
    padmask = P.sb([128, NTOK // 128], F32, "padmask")
    P.dma(SY, padmask[:], padmask_d)
    banks = [P.ps([128, 512], F32, f"bank{i}") for i in range(8)]

    def bview(bank_i, dtype, pattern=None, **kw):
        ap = banks[bank_i][:]
        if dtype is BF16:
            ap = ap.bitcast(BF16)
        if pattern:
            ap = ap.rearrange(pattern, **kw)
        return ap

    def mixer(l, src, dst, first_out_tile):
        with P.scope():
            WIN = P.sb([128, 8, NIN], BF16, "WIN")
            for c in range(8):
                for c0 in range(0, NIN, 640):
                    P.dma(G, WIN[:, c, c0:c0 + 640], w_in[l, c * 128:(c + 1) * 128, c0:c0 + 640], writes=[("WIN", c)])
            WINk = [("WIN", c) for c in range(8)]
            WOUT = P.sb([128, 8, D], BF16, "WOUT")
            for c in range(8):
                P.dma(G, WOUT[:, c, :], w_out[l, c * 128:(c + 1) * 128, :], writes=["WOUT"])
            G1 = P.sb([128, D], F32, "G1")
            P.dma(SY, G1[:], norm1_g[l].partition_broadcast(128))
            GNG = P.sb([64, 512], F32, "GNG")
            GNB = P.sb([64, 512], F32, "GNB")
            P.dma(SY, GNG[:], rwkv_gn_g[l].partition_broadcast(64))
            P.dma(SY, GNB[:], rwkv_gn_b[l].partition_broadcast(64))
            CMASK4 = P.sb([128, 4, TT], F32, "CMASK4")
            for h_ in range(4):
                P.dma(SY, CMASK4[:, h_, :], cmask_d, writes=[CMASK4])
            PV = P.sb([128, 64], F32, "PV")
            MU, OMU, W0, A0, KKc, KAc, OMKA, V0c, RKc, PSC, CB, LNG, LNB = 0, 14, 28, 32, 36, 40, 44, 48, 52, 56, 58, 60, 62
            CW = P.sb([128, 2, 31], F32, "CW")
            DIAG = P.sb([128, 2, 31, 128], BF16, "DIAG")
            PW = P.sb([128, 2, 128], BF16, "PW")
            with P.scope():
                PVR = P.sb([64, 128], F32, "PVR")
                P.memset(V, PVR[:], 0.0)

                def pvec(col, src_ap, n):
                    P.dma(SY, PVR[col:col + n, :], src_ap.rearrange("(c p) -> c p", p=128), writes=[PVR])
                pvec(MU, shift_mu[l], 14)
                pvec(W0, rwkv_w0[l], 4)
                pvec(A0, rwkv_a0[l], 4)
                pvec(KKc, rwkv_k_k[l], 4)
                pvec(KAc, rwkv_k_a[l], 4)
                if l == 1:
                    pvec(V0c, rwkv_v0[0], 4)
                pvec(RKc, rwkv_r_k[l], 4)
                pvec(PSC, pool_scale[l], 2)
                pvec(CB, conv_b[l], 2)
                pvec(LNG, conv_ln_g[l], 2)
                pvec(LNB, conv_ln_b[l], 2)
                P.tr(banks[0][:, 0:64], PVR[:], identf[0:64, 0:64])
                P.cp(V, PV[:], banks[0][:, 0:64])
                CWR = P.sb([31, 256], F32, "CWR")
                P.dma(SY, CWR[:], conv_w[l])
                for t in range(2):
                    P.tr(banks[1][:, t * 32:t * 32 + 31], CWR[:, t * 128:(t + 1) * 128], identf[0:31, 0:31])
                P.cp(V, CW[:], banks[1][:, 0:64].rearrange("p (t j) -> p t j", j=32)[:, :, 0:31])
                PWf = P.sb([128, 2, 128], F32, "PWf")
                P.memset(V, PWf[:], 0.0)
                for t in range(2):
                    P.dma(SY, PWf[0:64, t, 0:64], pool_w[l, 2 * t], writes=[PWf])
                    P.dma(SY, PWf[64:128, t, 64:128], pool_w[l, 2 * t + 1], writes=[PWf])
                P.cp(V, PW[:], PWf[:])
            P.ts(V, PV[:, OMU:OMU + 14], PV[:, MU:MU + 14], -1.0, 1.0, ALU.mult, ALU.add)
            P.ts(V, PV[:, OMKA:OMKA + 4], PV[:, KAc:KAc + 4], -1.0, 1.0, ALU.mult, ALU.add)
            for t in range(2):
                for j in range(31):
                    P.ts(V if j % 2 else G, DIAG[:, t, j, :], identf[:], CW[:, t, j:j + 1], None, ALU.mult)
            PFIX = P.sb([128, 2, 2, 16], F32, "PFIX")
            P.dma(SY, PFIX[:], pfix_d)
            W2A2 = P.sb([128, 512], BF16, "W2A2")
            P.dma(G, W2A2[0:64, :], rwkv_w2[l], writes=[W2A2])
            P.dma(G, W2A2[64:128, :], rwkv_a2[l], writes=[W2A2])
            G2 = P.sb([128, 512], BF16, "G2")
            P.dma(G, G2[:], rwkv_g2[l])
            if l == 1:
                V1 = P.sb([128, 4, 32], BF16, "V1")
                P.dma(G, V1[:], rwkv_v1[0].rearrange("(c p) r -> p c r", p=128))
                V2 = P.sb([32, 512], BF16, "V2")
                P.dma(G, V2[:], rwkv_v2[0])
            ONES256 = P.sb([128, 128], F32, "ONES256")
            P.memset(V, ONES256[:], 1.0 / 256.0)
            BLK = P.sb([128, 128], BF16, "BLK")
            HSEL = P.sb([128, 4, 8], BF16, "HSEL")
            with P.scope():
                BLKf = P.sb([128, 128], F32, "BLKf")
                P.dma(SY, BLKf[:], blockones_d)
                P.cp(V, BLK[:], BLKf[:])
                HSELf = P.sb([128, 4, 8], F32, "HSELf")
                P.dma(SY, HSELf[:], headsel_d)
                P.cp(V, HSEL[:], HSELf[:])
            MASKAR = P.sb([64, 128], F32, "MASKAR")
            MASKLOW = P.sb([64, 64], F32, "MASKLOW")
            I8 = P.sb([64, 64], F32, "I8")
            CMASK = P.sb([128, TT], F32, "CMASK")
            P.dma(SY, MASKAR[:], maskar_d)
            P.dma(SY, MASKLOW[:], masklow_d)
            P.dma(SY, I8[:], i8_d)
            P.dma(SY, CMASK[:], cmask_d)

            chk("m0w")
            def dbl(shape, dt, name):
                return [P.sb(shape, dt, f"{name}{b}") for b in range(2)]
            HXs = dbl([128, NSUB, D], F32, "HX")
            SQ = P.sb([128, D], BF16, "SQ")
            SS = P.sb([128, NSUB], F32, "SS")
            RS = P.sb([128, NSUB], F32, "RS")
            XN = P.sb([128, NSUB, D], BF16, "XN")
            HNT = P.sb([128, 8, TT], BF16, "HNT")
            UP = P.sb([128, 2, 16 + TT], F32, "UP")
            S1 = P.sb([128, 2, 16 + TT], F32, "S1")
            S2 = P.sb([128, 2, 16 + TT], F32, "S2")
            S3 = P.sb([128, 2, 16 + TT], F32, "S3")
            S16 = P.sb([128, 16 + TT], F32, "S16")
            DP = P.sb([128, 2, TT], BF16, "DP")
            CA = P.sb([128, 2, TT], F32, "CA")
            SG = P.sb([128, TT], F32, "SG")
            U = P.sb([128, 2, 32 + TT], BF16, "U")
            YB = P.sb([128, 2, TT], F32, "YB")
            YSQ = P.sb([128, 2, TT], F32, "YSQ")
            MEAN = P.sb([128, TT], F32, "MEAN")
            VAR = P.sb([128, TT], F32, "VAR")
            RSTD = P.sb([128, TT], F32, "RSTD")
            CT = P.sb([128, TT], F32, "CT")
            HALO = P.sb([128, 14], F32, "HALO")
            TMPM4 = P.sb([128, 4, TT], F32, "TMPM4")
            CT2 = P.sb([128, 2, TT], F32, "CT2")
            CF = P.sb([128, 12, TT], F32, "CF")
            ER = P.sb([128, 4, TT], F32, "ER")
            SGW4 = P.sb([128, 4, TT], F32, "SGW4")
            CUM4 = P.sb([128, 4, TT], F32, "CUM4")
            EA4 = P.sb([128, 4, TT], F32, "EA4")
            EI4 = P.sb([128, 4, TT], F32, "EI4")
            AA4 = P.sb([128, 4, TT], F32, "AA4")
            KK4 = P.sb([128, 4, TT], F32, "KK4")
            KSQ4 = P.sb([128, 4, TT], BF16, "KSQ4")
            RN4 = P.sb([128, 4, TT], F32, "RN4")
            KF4 = P.sb([128, 4, TT], F32, "KF4")
            BETA4 = P.sb([128, 4, TT], F32, "BETA4")
            VFT = P.sb([128, 4, TT], F32, "VFT") if l == 1 else None
            T1 = P.sb([32, TT], BF16, "T1") if l == 1 else None
            BH = P.sb([128, 4, TT], BF16, "BH")
            KH = P.sb([128, 4, TT], BF16, "KH")
            VB = P.sb([128, 4, TT], BF16, "VB")
            LACTs = dbl([128, 2, TT], BF16, "LACT")
            ARs = dbl([128, 4, NCH, 128], BF16, "AR")
            BTs = dbl([128, 4, TT], BF16, "BT")
            KTs = dbl([128, 4, TT], BF16, "KT")
            RKBs = dbl([128, 4, TT], BF16, "RKB")
            ARos = dbl([64, 4, NCH, 128], BF16, "ARo")
            BTos = dbl([64, 4, TT], BF16, "BTo")
            KTos = dbl([64, 4, TT], BF16, "KTo")
            ECos = dbl([64, 4, NCH], F32, "ECo")
            ECes = dbl([128, 4, NCH], F32, "ECe")
            VTOKs = dbl([64, NCH, 512], BF16, "VTOK")
            BHTs = dbl([64, NCH, 512], BF16, "BHT")
            KHTs = dbl([64, NCH, 512], BF16, "KHT")
            YMIXs = dbl([128, 8, TT], BF16, "YMIX")
            MBs = P.sb([64, 8, 128], BF16, "MBs")
            MKs = P.sb([64, 8, 128], BF16, "MKs")
            Mp = [P.sb([64, 8, 64], BF16, f"Mp{i}") for i in range(2)]
            Np = [P.sb([64, 8, 64], BF16, f"Np{i}") for i in range(2)]
            Tb = P.sb([64, 8, 64], BF16, "Tb")
            WTs = P.sb([64, 8, 64], BF16, "WTs")
            UTs = P.sb([64, 8, 64], BF16, "UTs")
            Sf = P.sb([64, 8, 64], F32, "Sf")
            Sb = P.sb([64, 8, 64], BF16, "Sb")
            YS = P.sb([64, 8, 64], F32, "YS")
            YQ = P.sb([64, 8, 64], F32, "YQ")
            ST = P.sb([64, 40], F32, "ST")
            BON = P.sb([64, 8], F32, "BON")
            YM = P.sb([64, 512], BF16, "YM")

            P.memset(V, UP[:], 0.0)
            P.memset(V, U[:], 0.0)
            P.memset(V, HALO[:], 0.0)
            P.memset(V, Sf[:], 0.0)
            P.memset(V, Sb[:], 0.0)
            for b_ in range(2):
                P.memset(G, ARs[b_][:], 0.0)
                P.memset(G, LACTs[b_][:], 0.0)

            def prep(it):
                b = it % 2
                t0 = it * TT
                lite = it < first_out_tile - 1
                HX, LACT, AR, BT, KT, RKB = HXs[b], LACTs[b], ARs[b], BTs[b], KTs[b], RKBs[b]
                ARo, BTo, KTo, ECo, ECe = ARos[b], BTos[b], KTos[b], ECos[b], ECes[b]
                VTOK, BHT, KHT, YMIX = VTOKs[b], BHTs[b], KHTs[b], YMIXs[b]
                ymn = "YMIX%d" % b
                arn, btn, ktn = "AR%d" % b, "BT%d" % b, "KT%d" % b
                P.dma(SY, HX[:], src[t0:t0 + TT, :].rearrange("(s p) d -> p s d", p=128))
                for s in range(NSUB):
                    P.act(SQ[:], HX[:, s, :], AF.Square, accum_out=SS[:, s:s + 1])
                    P.act(RS[:, s:s + 1], SS[:, s:s + 1], AF.Sqrt, bias=1e-6, scale=1.0 / D)
                    P.op(V, lambda e, s=s: e.reciprocal(out=RS[:, s:s + 1], in_=RS[:, s:s + 1]), [RS], [RS])
                    P.stt(XN[:, s, :], HX[:, s, :], RS[:, s:s + 1], G1[:], ALU.mult, ALU.mult)
                    yield
                    pT = bview(7, BF16, "p (a b) -> p a b", b=128)
                    for c in range(8):
                        P.tr(pT[:, c, :], XN[:, s, c * 128:(c + 1) * 128], identb[:], w=[banks[7]])
                    P.cp(V, HNT[:, :, s * 128:(s + 1) * 128], pT, r=[banks[7]])
                    yield

                def proj(f, pb):
                    pm = banks[pb][:, 0:TT]
                    for c in range(8):
                        P.mm(pm, WIN[:, c, f * 128:(f + 1) * 128], HNT[:, c, :], start=(c == 0), stop=(c == 7),
                             r=[("WIN", c), HNT], w=[banks[pb]])
                    return pm

                if not lite:
                    for t in range(2):
                        pm = proj(t, t % 2)
                        P.cp(A, UP[:, t, 16:16 + TT], pm, r=[banks[t % 2]], w=[UP])
                        yield
                    W_ = 16 + TT
                    P.tt(V, S1[:, :, 1:W_], UP[:, :, 1:W_], UP[:, :, 0:W_ - 1], ALU.add)
                    P.tt(G, S2[:, :, 3:W_], S1[:, :, 3:W_], S1[:, :, 1:W_ - 2], ALU.add)
                    yield
                    P.tt(V, S3[:, :, 7:W_], S2[:, :, 7:W_], S2[:, :, 3:W_ - 4], ALU.add)
                    P.tt(G, S16[:, 15:W_], S3[:, 1, 15:W_], S3[:, 1, 7:W_ - 8], ALU.add)
                    yield
                    grp = [(0, 0, S1[0:64, 0, :], 2.0), (0, 64, S2[64:128, 0, :], 4.0), (1, 0, S3[0:64, 1, :], 8.0), (1, 64, S16[64:128, :], 16.0)]
                    for (t, p0, sbuf, win) in grp:
                        for which, tile_idx in ((0, 0), (1, NTILE // 2)):
                            if it == tile_idx:
                                P.tt(V, sbuf[:, 16:32], sbuf[:, 16:32], PFIX[p0:p0 + 64, t, which, :], ALU.mult)
                        P.stt(DP[p0:p0 + 64, t, :], sbuf[:, 16:W_], 1.0 / win, UP[p0:p0 + 64, t, 16:W_], ALU.mult, ALU.subtract)
                    yield
                    P.cp(G, UP[:, :, 0:16], UP[:, :, TT:TT + 16])
                    for t in range(2):
                        pm2 = banks[t % 2][:, 0:TT]
                        P.mm(pm2, PW[:, t, :], DP[:, t, :], w=[banks[t % 2]])
                        P.act(YMIX[:, t, :], pm2, AF.Identity, scale=PV[:, PSC + t:PSC + t + 1], r=[banks[t % 2], PV], w=[(ymn, t)])
                    yield
                    for t in range(2):
                        pm = proj(2 + t, t % 2)
                        P.cp(A, CA[:, t, :], pm, r=[banks[t % 2]], w=[CA])
                        yield
                    for t in range(2):
                        pm = proj(4 + t, t % 2)
                        P.act(SG[:], pm, AF.Sigmoid, r=[banks[t % 2]])
                        P.tt(V, U[:, t, 32:32 + TT], CA[:, t, :], SG[:], ALU.mult)
                        yield
                    for t in range(2):
                        pc = banks[t % 2][:, 0:TT]
                        for j in range(31):
                            P.mm(pc, DIAG[:, t, j, :], U[:, t, 2 + j:2 + j + TT], start=(j == 0), stop=(j == 30), w=[banks[t % 2]])
                            if j % 8 == 7:
                                yield
                        P.act(YB[:, t, :], pc, AF.Identity, bias=PV[:, CB + t:CB + t + 1], r=[banks[t % 2], PV], w=[YB])
                        P.act(YSQ[:, t, :], pc, AF.Square, bias=PV[:, CB + t:CB + t + 1], r=[banks[t % 2], PV], w=[YSQ])
                        yield
                    P.cp(G, U[:, :, 0:32], U[:, :, TT:TT + 32])
                    pmean = banks[0][:, 0:TT]
                    psq = banks[1][:, 0:TT]
                    for t in range(2):
                        P.mm(pmean, ONES256[:], YB[:, t, :], start=(t == 0), stop=(t == 1), w=[banks[0]])
                    for t in range(2):
                        P.mm(psq, ONES256[:], YSQ[:, t, :], start=(t == 0), stop=(t == 1), w=[banks[1]])
                    P.cp(A, MEAN[:], pmean, r=[banks[0]])
                    P.act(VAR[:], pmean, AF.Square, r=[banks[0]])
                    yield
                    P.tt(V, VAR[:], psq, VAR[:], ALU.subtract, r=[banks[1], VAR])
                    P.act(RSTD[:], VAR[:], AF.Sqrt, bias=1e-5)
                    P.op(V, lambda e: e.reciprocal(out=RSTD[:], in_=RSTD[:]), [RSTD], [RSTD])
                    yield
                    for t in range(2):
                        P.tt(V, CT[:], YB[:, t, :], MEAN[:], ALU.subtract)
                        P.tt(V, CT[:], CT[:], RSTD[:], ALU.mult)
                        P.act(YMIX[:, 2 + t, :], CT[:], AF.Silu, bias=PV[:, LNB + t:LNB + t + 1], scale=PV[:, LNG + t:LNG + t + 1],
                              w=[(ymn, 2 + t)])
                        yield
                groups = [[4, 5, 6, 7], [8, 9, 10, 11], [12]] if lite else [[0, 1, 2, 3], [4, 5, 6, 7], [8, 9, 10, 11], [12, 13]]
                for gi, grp_ in enumerate(groups):
                    pb = gi % 2
                    n = len(grp_)
                    q0 = grp_[0]
                    pm4 = banks[pb][:, 0:4 * TT].rearrange("p (h t) -> p h t", t=TT)[:, 0:n, :]
                    for i_, q in enumerate(grp_):
                        for c in range(8):
                            P.mm(pm4[:, i_, :], WIN[:, c, (6 + q) * 128:(7 + q) * 128], HNT[:, c, :], start=(c == 0), stop=(c == 7),
                                 r=[("WIN", c), HNT], w=[banks[pb]])
                        if i_ % 2 == 1:
                            yield
                    mu4 = PV[:, MU + q0:MU + q0 + n].unsqueeze(2)
                    omu4 = PV[:, OMU + q0:OMU + q0 + n].unsqueeze(2)
                    hal = HALO[:, q0:q0 + n].unsqueeze(2)
                    P.tt(V, TMPM4[:, 0:n, 1:TT], pm4[:, :, 0:TT - 1], mu4.to_broadcast([128, n, TT - 1]), ALU.mult, r=[banks[pb], PV], w=[TMPM4])
                    P.tt(G, TMPM4[:, 0:n, 0:1], hal, mu4, ALU.mult, r=[HALO, PV], w=[TMPM4])
                    P.cp(A, hal, pm4[:, :, TT - 1:TT], r=[banks[pb]], w=[HALO])
                    if q0 < 12:
                        dstv = CF[:, q0:q0 + n, :]
                        wk = [("CF", q) for q in grp_]
                    else:
                        dstv = CT2[:, 0:n, :]
                        wk = [CT2]
                    P.tt(V, dstv, pm4, omu4.to_broadcast([128, n, TT]), ALU.mult, r=[banks[pb], PV], w=wk)
                    P.tt(V, dstv, dstv, TMPM4[:, 0:n, :], ALU.add, r=wk + [TMPM4], w=wk)
                    yield
                    if q0 == 12:
                        P.act(LACT[0:64, 0, :], CT2[0:64, 0, :], AF.Tanh, w=[LACT])
                        P.cp(V, LACT[64:128, 0, :], CT2[64:128, 0, :], w=[LACT])
                        if not lite:
                            P.act(LACT[:, 1, :], CT2[:, 1, :], AF.Sigmoid, w=[LACT])
                        yield
                CFr = [("CF", q) for q in range(12)]
                if l == 0:
                    P.dma(SY, vfirst[:, t0:t0 + TT].rearrange("(c p) t -> p c t", p=128), CF[:, 8:12, :], reads=CFr, writes=["vfirst"])
                    for hp in range(4):
                        P.cp(G, VB[:, hp, :], CF[:, 8 + hp, :], r=CFr, w=[("VB", hp)])
                    yield
                else:
                    P.dma(SY, VFT[:], vfirst[:, t0:t0 + TT].rearrange("(c p) t -> p c t", p=128), reads=["vfirst"], writes=[VFT])
                    for hp in range(4):
                        P.cp(G, VB[:, hp, :], CF[:, 8 + hp, :], r=CFr, w=[("VB", hp)])
                    yield
                    pv1 = banks[0][0:32, 0:TT]
                    for hp in range(4):
                        P.mm(pv1, V1[:, hp, :], VB[:, hp, :], start=(hp == 0), stop=(hp == 3), r=[V1] + [("VB", h) for h in range(4)], w=[banks[0]])
                    P.cp(A, T1[:], pv1, r=[banks[0]])
                    yield
                    for hp in range(4):
                        pv2 = banks[1][:, 0:TT]
                        P.mm(pv2, V2[0:32, hp * 128:(hp + 1) * 128], T1[:], w=[banks[1]])
                        P.act(SG[:], pv2, AF.Sigmoid, bias=PV[:, V0c + hp:V0c + hp + 1], r=[banks[1], PV])
                        P.tt(V, CT[:], VFT[:, hp, :], CF[:, 8 + hp, :], ALU.subtract, r=[VFT] + CFr)
                        P.tt(V, CT[:], CT[:], SG[:], ALU.mult)
                        P.tt(V, CF[:, 8 + hp, :], CF[:, 8 + hp, :], CT[:], ALU.add, r=CFr + [CT], w=[("CF", 8 + hp)])
                        yield
                    for hp in range(4):
                        P.cp(G, VB[:, hp, :], CF[:, 8 + hp, :], r=CFr, w=[("VB", hp)])
                    yield
                def flat(t_):
                    return t_[:].rearrange("p h t -> p (h t)")

                def c3(ap_):
                    return ap_.rearrange("p h (c t) -> p (h c) t", t=64)
                ERk = [("ER", h) for h in range(4)]
                pw4 = banks[0][:, 0:4 * TT].rearrange("p (h t) -> p h t", t=TT)
                pa4 = banks[1][:, 0:4 * TT].rearrange("p (h t) -> p h t", t=TT)
                for hp in range(4):
                    fs = slice(hp * 128, (hp + 1) * 128)
                    P.mm(pw4[:, hp, :], W2A2[0:64, fs], LACT[0:64, 0, :], w=[banks[0]])
                for hp in range(4):
                    fs = slice(hp * 128, (hp + 1) * 128)
                    P.mm(pa4[:, hp, :], W2A2[64:128, fs], LACT[64:128, 0, :], w=[banks[1]])
                yield
                for hp in range(4):
                    P.act(SGW4[:, hp, :], pw4[:, hp, :], AF.Sigmoid, bias=PV[:, W0 + hp:W0 + hp + 1], r=[banks[0], PV], w=[SGW4])
                yield
                for hp in range(4):
                    P.act(AA4[:, hp, :], pa4[:, hp, :], AF.Sigmoid, bias=PV[:, A0 + hp:A0 + hp + 1], r=[banks[1], PV], w=[AA4])
                yield
                P.op(V, lambda e: e.tensor_tensor_scan(out=flat(CUM4), data0=flat(CMASK4), data1=flat(SGW4), initial=0.0, op0=ALU.mult, op1=ALU.add),
                     [CMASK4, SGW4], [CUM4])
                P.tt(G, flat(EA4), flat(CUM4), flat(SGW4), ALU.subtract)
                P.act(flat(ER), flat(CUM4), AF.Exp, scale=-CDEC, r=[CUM4], w=ERk)
                yield
                P.act(flat(EA4), flat(EA4), AF.Exp, scale=-CDEC)
                P.act(flat(EI4), flat(CUM4), AF.Exp, scale=CDEC)
                kk4 = CF[:, 4:8, :]
                P.tt(V, KK4[:], kk4, PV[:, KKc:KKc + 4].unsqueeze(2).to_broadcast([128, 4, TT]), ALU.mult, r=CFr + [PV], w=[KK4])
                yield
                P.act(flat(KSQ4), flat(KK4), AF.Square)
                pss4 = banks[0][:, 0:4 * TT].rearrange("p (h t) -> p h t", t=TT)
                for hp in range(4):
                    P.mm(pss4[:, hp, :], BLK[:], KSQ4[:, hp, :], w=[banks[0]])
                P.act(flat(RN4), banks[0][:, 0:4 * TT], AF.Sqrt, bias=1e-24, r=[banks[0]], w=[RN4])
                EH4 = SGW4
                P.tt(V, c3(EH4[:]), c3(EI4[:]), c3(ER[:])[:, :, 63:64].to_broadcast([128, 4 * NCH, 64]), ALU.mult, r=[EI4] + ERk, w=[SGW4])
                yield
                P.op(V, lambda e: e.reciprocal(out=flat(RN4), in_=flat(RN4)), [RN4], [RN4])
                P.tt(V, flat(KK4), flat(KK4), flat(RN4), ALU.mult)
                TM24 = RN4
                P.tt(V, TM24[:], AA4[:], PV[:, KAc:KAc + 4].unsqueeze(2).to_broadcast([128, 4, TT]), ALU.mult, r=[AA4, PV, KK4], w=[RN4])
                P.tt(G, TM24[:], TM24[:], PV[:, OMKA:OMKA + 4].unsqueeze(2).to_broadcast([128, 4, TT]), ALU.add, r=[RN4, PV], w=[RN4])
                yield
                P.tt(V, KF4[:], kk4, TM24[:], ALU.mult, r=CFr + [RN4], w=[KF4])
                P.tt(G, flat(BETA4), flat(AA4), flat(KK4), ALU.mult)
                yield
                AR8 = AR[:].rearrange("p h c x -> p (h c) x")
                P.stt(AR8[:, :, 0:64], c3(KK4[:]), -1.0, c3(EA4[:]), ALU.mult, ALU.mult, r=[KK4, EA4], w=[AR])
                if not lite:
                    P.tt(V, AR8[:, :, 64:128], c3(CF[:, 0:4, :]), c3(ER[:]), ALU.mult, r=CFr + ERk, w=[AR])
                yield
                P.tt(V, flat(BT), flat(BETA4), flat(EI4), ALU.mult)
                P.tt(G, flat(KT), flat(KF4), flat(EI4), ALU.mult)
                yield
                P.tt(V, flat(BH), flat(BETA4), flat(EH4), ALU.mult, r=[BETA4, SGW4], w=[("BH", h) for h in range(4)])
                P.tt(G, flat(KH), flat(KF4), flat(EH4), ALU.mult, r=[KF4, SGW4], w=[("KH", h) for h in range(4)])
                if not lite:
                    P.tt(V, RKB[:], CF[:, 0:4, :], PV[:, RKc:RKc + 4].unsqueeze(2).to_broadcast([128, 4, TT]), ALU.mult, r=CFr + [PV], w=[RKB])
                    P.tt(V, flat(RKB), flat(RKB), flat(KF4), ALU.mult)
                yield
                ARk = [AR]
                BTk = [BT]
                KTk = [KT]
                P.dma(SY, ARo[:], AR[64:128, :, :, :], reads=ARk, writes=[ARo])
                P.dma(SY, BTo[:], BT[64:128, :, :], reads=BTk, writes=[BTo])
                P.dma(SY, KTo[:], KT[64:128, :, :], reads=KTk, writes=[KTo])
                ER4 = ER[:].rearrange("p h (c t) -> p h c t", t=64)
                P.cp(V, ECe[:], ER4[:, :, :, 63], r=[("ER", h) for h in range(4)], w=[ECe])
                P.dma(SY, ECo[:], ECe[64:128, :, :], reads=[ECe], writes=[ECo])
                yield
                for c in range(NCH):
                    cs = slice(c * 64, (c + 1) * 64)
                    for (srcT, dstT, kn) in ((VB, VTOK, "VB"), (BH, BHT, "BH"), (KH, KHT, "KH")):
                        pT = bview(7, BF16, "p (a b) -> p a b", b=128)
                        for hp in range(4):
                            P.tr(pT[0:64, hp, :], srcT[:, hp, cs], identb[:], r=[(kn, hp), identb], w=[banks[7]])
                        P.cp(A, dstT[:, c, :].rearrange("p (a b) -> p a b", b=128), pT[0:64, 0:4, :], r=[banks[7]], w=[dstT])
                        yield

            def scan(it):
                b = it % 2
                t0 = it * TT
                lite = it < first_out_tile - 1
                HX, LACT, AR, BT, KT, RKB = HXs[b], LACTs[b], ARs[b], BTs[b], KTs[b], RKBs[b]
                ARo, BTo, KTo, ECo, ECe = ARos[b], BTos[b], KTos[b], ECos[b], ECes[b]
                VTOK, BHT, KHT, YMIX = VTOKs[b], BHTs[b], KHTs[b], YMIXs[b]
                ymn = "YMIX%d" % b
                arn, btn, ktn = "AR%d" % b, "BT%d" % b, "KT%d" % b
                ARk = [AR]
                BTk = [BT]
                KTk = [KT]

                def fm(even, odd, h):
                    return (even[0:64, h // 2] if h % 2 == 0 else odd[0:64, h // 2])
                mar = MASKAR[:].unsqueeze(1).to_broadcast([64, 4, 128])
                for c in range(NCH):
                    cs = slice(c * 64, (c + 1) * 64)
                    rdk = ARk + BTk + KTk + [ARo, BTo, KTo]
                    pMa = bview(2, F32, "p (h x) -> p h x", x=128)
                    pMb = bview(3, F32, "p (h x) -> p h x", x=128)

                    def pM(h):
                        return (pMa if h < 4 else pMb)[0:64, h % 4, :]
                    pMk = [banks[2], banks[3]]
                    for h in range(8):
                        P.mm(pM(h), fm(BT, BTo, h)[:, cs], fm(AR, ARo, h)[:, c, :], r=rdk, w=[pMk[h // 4]])
                    P.tt(V, MBs[:, 0:4, :], pMa[0:64], mar, ALU.mult, r=[banks[2], MASKAR], w=[MBs])
                    P.tt(V, MBs[:, 4:8, :], pMb[0:64], mar, ALU.mult, r=[banks[3], MASKAR], w=[MBs])
                    yield
                    for h in range(8):
                        P.mm(pM(h), fm(KT, KTo, h)[:, cs], fm(AR, ARo, h)[:, c, :], r=rdk, w=[pMk[h // 4]])
                    P.tt(V, MKs[:, 0:4, :], pMa[0:64], mar, ALU.mult, r=[banks[2], MASKAR], w=[MKs])
                    P.tt(V, MKs[:, 4:8, :], pMb[0:64], mar, ALU.mult, r=[banks[3], MASKAR], w=[MKs])
                    yield
                    pC = bview(4, F32, "p (h x) -> p h x", x=64)
                    pC2 = bview(2, F32, "p (h x) -> p h x", x=64)
                    pC3 = bview(3, F32, "p (h x) -> p h x", x=64)
                    for h in range(8):
                        P.mm(pC[0:64, h, :], fm(AR, ARo, h)[:, c, 0:64], fm(BT, BTo, h)[:, cs], r=rdk, w=[banks[4]])
                    P.tt(V, Np[0][:], pC[0:64], MASKLOW[:].unsqueeze(1).to_broadcast([64, 8, 64]), ALU.mult, r=[banks[4], MASKLOW])
                    P.cp(G, Mp[0][:], MBs[:, :, 0:64])
                    P.tt(G, Tb[:], MBs[:, :, 0:64], I8[:].unsqueeze(1).to_broadcast([64, 8, 64]), ALU.add, r=[MBs, I8])
                    yield
                    cur = 0
                    for step in range(5):
                        nxt = 1 - cur
                        last = (step == 4)
                        for h in range(8):
                            P.mm(pC[0:64, h, :], Mp[cur][:, h, :], Np[cur][:, h, :], w=[banks[4]])
                        P.cp(A, Np[nxt][:], pC[0:64], r=[banks[4]])
                        if not last:
                            for h in range(8):
                                P.mm(pC2[0:64, h, :], Np[cur][:, h, :], Mp[cur][:, h, :], w=[banks[2]])
                            P.cp(V, Mp[nxt][:], pC2[0:64], r=[banks[2]])
                        yield
                        for h in range(8):
                            P.mm(pC3[0:64, h, :], Np[nxt][:, h, :], Tb[:, h, :], w=[banks[3]])
                        P.tt(V, Tb[:], Tb[:], pC3[0:64], ALU.add, r=[Tb, banks[3]])
                        cur = nxt
                        yield
                    pZ = bview(5, F32, "p (h x) -> p h x", x=64)
                    for h in range(8):
                        hs = slice(h * 64, (h + 1) * 64)
                        P.mm(pZ[0:64, h, :], fm(AR, ARo, h)[:, c, 0:64], Sb[:, h, :], start=True, stop=False, r=rdk + [Sb], w=[banks[5]])
                        P.mm(pZ[0:64, h, :], MKs[:, h, 0:64], VTOK[:, c, hs], start=False, stop=True, w=[banks[5]])
                    P.cp(A, WTs[:], pZ[0:64], r=[banks[5]])
                    yield
                    for h in range(8):
                        P.mm(pZ[0:64, h, :], Tb[:, h, :], WTs[:, h, :], w=[banks[5]])
                    P.cp(V, UTs[:], pZ[0:64], r=[banks[5]])
                    yield
                    if not lite:
                        pY = bview(6, F32, "p (h x) -> p h x", x=64)
                        for h in range(8):
                            hs = slice(h * 64, (h + 1) * 64)
                            P.mm(pY[0:64, h, :], fm(AR, ARo, h)[:, c, 64:128], Sb[:, h, :], start=True, stop=False, r=rdk + [Sb], w=[banks[6]])
                            P.mm(pY[0:64, h, :], MBs[:, h, 64:128], UTs[:, h, :], start=False, stop=False, w=[banks[6]])
                            P.mm(pY[0:64, h, :], MKs[:, h, 64:128], VTOK[:, c, hs], start=False, stop=True, w=[banks[6]])
                        yield
                    for h in range(8):
                        hs = slice(h * 64, (h + 1) * 64)
                        P.mm(pZ[0:64, h, :], BHT[:, c, hs], UTs[:, h, :], start=True, stop=False, w=[banks[5]])
                        P.mm(pZ[0:64, h, :], KHT[:, c, hs], VTOK[:, c, hs], start=False, stop=True, w=[banks[5]])
                    for par, ecsrc in ((0, ECe[0:64, :, c:c + 1]), (1, ECo[:, :, c:c + 1])):
                        Sv = Sf[:].rearrange("p (h two) x -> p h two x", two=2)[:, :, par, :]
                        P.tt(V, Sv, Sv, ecsrc.to_broadcast([64, 4, 64]), ALU.mult, r=[Sf, ECo, ECe], w=[Sf])
                    P.tt(V, Sf[:], Sf[:], pZ[0:64], ALU.add, r=[Sf, banks[5]])
                    P.cp(A, Sb[:], Sf[:])
                    yield
                    if lite:
                        continue
                    P.cp(A, YS[:], pY[0:64], r=[banks[6]])
                    P.act(YQ[:], pY[0:64], AF.Square, r=[banks[6]])
                    P.op(V, lambda e: e.tensor_reduce(out=ST[:, 0:8], in_=YS[:], axis=AX.X, op=ALU.add), [YS], [ST])
                    P.op(V, lambda e: e.tensor_reduce(out=ST[:, 8:16], in_=YQ[:], axis=AX.X, op=ALU.add), [YQ], [ST])
                    yield
                    P.ts(V, ST[:, 16:24], ST[:, 0:8], 1.0 / 64, None, ALU.mult)
                    P.tt(V, ST[:, 24:32], ST[:, 16:24], ST[:, 16:24], ALU.mult)
                    P.stt(ST[:, 32:40], ST[:, 8:16], 1.0 / 64, ST[:, 24:32], ALU.mult, ALU.subtract)
                    P.act(ST[:, 32:40], ST[:, 32:40], AF.Sqrt, bias=64e-5)
                    P.op(V, lambda e: e.reciprocal(out=ST[:, 32:40], in_=ST[:, 32:40]), [ST], [ST])
                    yield
                    P.tt(V, YS[:], YS[:], ST[:, 16:24].unsqueeze(2).to_broadcast([64, 8, 64]), ALU.subtract, r=[YS, ST])
                    P.tt(V, YS[:], YS[:], ST[:, 32:40].unsqueeze(2).to_broadcast([64, 8, 64]), ALU.mult, r=[YS, ST])
                    YS2 = YS[:].rearrange("p h x -> p (h x)")
                    P.tt(V, YS2, YS2, GNG[:], ALU.mult, r=[YS, GNG], w=[YS])
                    P.tt(G, YS2, YS2, GNB[:], ALU.add, r=[YS, GNB], w=[YS])
                    yield
                    pBn = banks[3][0:64, 0:8]
                    for hp in range(4):
                        P.mm(pBn, RKB[:, hp, cs], HSEL[:, hp, :], start=(hp == 0), stop=(hp == 3), w=[banks[3]])
                    P.cp(A, BON[:], pBn, r=[banks[3]])
                    P.tt(V, YQ[:], VTOK[:, c, :].rearrange("p (h x) -> p h x", x=64), BON[:].unsqueeze(2).to_broadcast([64, 8, 64]), ALU.mult,
                         r=[VTOK, BON], w=[YQ])
                    P.tt(V, YS[:], YS[:], YQ[:], ALU.add)
                    yield
                    pG = banks[2][0:64, :]
                    P.mm(pG, LACT[:, 1, cs], G2[:], w=[banks[2]])
                    P.tt(V, YM[:], YS2, pG, ALU.mult, r=[YS, banks[2]], w=[YM])
                    pT2 = bview(4, BF16, "p (a b) -> p a b", b=128)
                    for hp in range(4):
                        P.tr(pT2[:, hp, 0:64], YM[:, hp * 128:(hp + 1) * 128], identb[0:64, 0:64], r=[YM, identb], w=[banks[4]])
                    P.cp(A, YMIX[:, 4:8, cs], pT2[:, 0:4, 0:64], r=[banks[4]], w=[(ymn, "rw")])
                    yield
                if it >= first_out_tile:
                    ymk = [(ymn, i) for i in (0, 1, 2, 3, "rw")]
                    for s in range(NSUB):
                        for dh in range(2):
                            po = banks[2 + dh][:, :]
                            for fc in range(8):
                                P.mm(po, YMIX[:, fc, s * 128:(s + 1) * 128], WOUT[:, fc, dh * 512:(dh + 1) * 512],
                                     start=(fc == 0), stop=(fc == 7), r=ymk + [WOUT], w=[banks[2 + dh]])
                            P.tt(V, HX[:, s, dh * 512:(dh + 1) * 512], HX[:, s, dh * 512:(dh + 1) * 512], po, ALU.add, r=[HX, banks[2 + dh]], w=[HX])
                            yield
                    d0 = t0 - first_out_tile * TT
                    P.dma(SY, dst[d0:d0 + TT, :].rearrange("(s p) d -> p s d", p=128), HX[:], reads=[HX], writes=["dst%d" % l])
                yield

            for it in range(NTILE + 1):
                gens = []
                if it < NTILE:
                    gens.append(prep(it))
                if it >= 1:
                    gens.append(scan(it - 1))
                while gens:
                    for g_ in list(gens):
                        try:
                            next(g_)
                        except StopIteration:
                            gens.remove(g_)
                chk("m0T%d" % it)

    def ffn(l, src, src_row0, ntok, dst, dst_is_final):
        moe = (l == 1)
        E = NE if moe else 1
        F = DFE if moe else DFF
        FG = 256
        NFG = F // FG
        NJ = GT // 128
        TW = min(512, GT)
        NTT = GT // TW
        NSW = TW // 128
        with P.scope():
            G2n = P.sb([128, D], F32, "G2n")
            P.dma(SY, G2n[:], norm2_g[l].partition_broadcast(128))
            if dst_is_final:
                GF = P.sb([128, D], F32, "GF")
                P.dma(SY, GF[:], final_g.partition_broadcast(128))
            if moe:
                RT = P.sb([128, 8, NE], F32, "RT")
                P.dma(SY, RT[:], moe_router[0].rearrange("(c p) e -> p c e", p=128))
                XNF = P.sb([128, D], F32, "XNF")
                XNTF = P.sb([128, 8, 128], F32, "XNTF")
                LOG = P.sb([128, 8], F32, "LOG")
                MX = P.sb([128, 8], F32, "MX")
                G12 = P.sb([128, 2], F32, "G12")
                EQ = P.sb([128, 8], F32, "EQ")
                GATE = P.sb([128, NJ, NE], F32, "GATE")
            ACC = P.sb([128, NJ, D], F32, "ACC")
            HNT = P.sb([128, 8, GT], BF16, "HNT2")
            SQ = P.sb([128, D], F32, "SQ2")
            SS = P.sb([128, NJ], F32, "SS2")
            RS = P.sb([128, NJ], F32, "RS2")
            XN = P.sb([128, D], BF16, "XN2")
            WG = [P.sb([128, 8, FG], BF16, f"WG{i}") for i in range(2)]
            WU = [P.sb([128, 8, FG], BF16, f"WU{i}") for i in range(2)]
            WD = [P.sb([128, 2, D], BF16, f"WD{i}") for i in range(2)]
            SIL = [P.sb([128, TW], F32, f"SIL{i}") for i in range(2)]
            ACTT = [P.sb([128, 2, TW], BF16, f"ACTT{i}") for i in range(2)]
            for g in range(ntok // GT):
                r0 = src_row0 + g * GT
                for j in range(NJ):
                    P.dma(SY, ACC[:, j, :], src[r0 + j * 128:r0 + (j + 1) * 128, :], reads=["src%d" % l], writes=[("ACC", j)])
                for j in range(NJ):
                    ak = ("ACC", j)
                    P.act(SQ[:], ACC[:, j, :], AF.Square, accum_out=SS[:, j:j + 1], r=[ak], w=[SQ, SS])
                    P.act(RS[:, j:j + 1], SS[:, j:j + 1], AF.Sqrt, bias=1e-6, scale=1.0 / D)
                    P.op(V, lambda e, j=j: e.reciprocal(out=RS[:, j:j + 1], in_=RS[:, j:j + 1]), [RS], [RS])
                    P.stt(XN[:], ACC[:, j, :], RS[:, j:j + 1], G2n[:], ALU.mult, ALU.mult, r=[ak, RS, G2n])
                    pT = bview(7, BF16, "p (a b) -> p a b", b=128)
                    for c in range(8):
                        P.tr(pT[:, c, :], XN[:, c * 128:(c + 1) * 128], identb[:], w=[banks[7]])
                    P.cp(V, HNT[:, :, j * 128:(j + 1) * 128], pT, r=[banks[7]], w=[("HNT2", j // NSW)])
                    if moe:
                        P.stt(XNF[:], ACC[:, j, :], RS[:, j:j + 1], G2n[:], ALU.mult, ALU.mult, r=[ak, RS, G2n])
                        for half in range(2):
                            pTf = bview(5 + half, F32, "p (a b) -> p a b", b=128)
                            for c4 in range(4):
                                c = half * 4 + c4
                                P.tr(pTf[:, c4, :], XNF[:, c * 128:(c + 1) * 128], identf[:], w=[banks[5 + half]])
                            P.cp(A, XNTF[:, half * 4:half * 4 + 4, :], pTf, r=[banks[5 + half]], w=[XNTF])
                        pR = banks[4][:, 0:NE]
                        for c in range(8):
                            P.mm(pR, XNTF[:, c, :], RT[:, c, :], start=(c == 0), stop=(c == 7), w=[banks[4]])
                        P.cp(A, LOG[:], pR, r=[banks[4]])
                        P.op(V, lambda e: e.max(out=MX[:], in_=LOG[:]), [LOG], [MX])
                        P.tt(V, G12[:, 0:1], MX[:, 0:1], MX[:, 1:2], ALU.subtract, w=[G12])
                        P.act(G12[:, 1:2], G12[:, 0:1], AF.Sigmoid, scale=-1.0)
                        P.act(G12[:, 0:1], G12[:, 0:1], AF.Sigmoid)
                        P.ts(V, EQ[:], LOG[:], MX[:, 0:1], G12[:, 0:1], ALU.is_equal, ALU.mult)
                        P.ts(V, GATE[:, j, :], LOG[:], MX[:, 1:2], G12[:, 1:2], ALU.is_equal, ALU.mult, w=[GATE])
                        P.tt(V, GATE[:, j, :], GATE[:, j, :], EQ[:], ALU.add, r=[GATE, EQ], w=[GATE])
                widx = 0
                for e in range(E):
                    wg_d = (moe_w_gate[0, e] if moe else ffn_w_gate[0])
                    wu_d = (moe_w_up[0, e] if moe else ffn_w_up[0])
                    wd_d = (moe_w_down[0, e] if moe else ffn_w_down[0])
                    for fg in range(NFG):
                        wb = widx % 2
                        widx += 1
                        f0 = fg * FG
                        P.dma(G, WG[wb][:], wg_d[:, f0:f0 + FG].rearrange("(c p) f -> p c f", p=128))
                        P.dma(G, WU[wb][:], wu_d[:, f0:f0 + FG].rearrange("(c p) f -> p c f", p=128))
                        P.dma(G, WD[wb][:], wd_d[f0:f0 + FG, :].rearrange("(c p) d -> p c d", p=128))
                        for tt_ in range(NTT):
                            ab = tt_ % 2
                            for fc in range(2):
                                pG_ = banks[0 + 2 * fc][:, 0:TW]
                                pU_ = banks[1 + 2 * fc][:, 0:TW]
                                for c in range(8):
                                    P.mm(pG_, WG[wb][:, c, fc * 128:(fc + 1) * 128], HNT[:, c, tt_ * TW:(tt_ + 1) * TW],
                                         start=(c == 0), stop=(c == 7), r=[WG[wb], ("HNT2", tt_)], w=[banks[0 + 2 * fc]])
                                for c in range(8):
                                    P.mm(pU_, WU[wb][:, c, fc * 128:(fc + 1) * 128], HNT[:, c, tt_ * TW:(tt_ + 1) * TW],
                                         start=(c == 0), stop=(c == 7), r=[WU[wb], ("HNT2", tt_)], w=[banks[1 + 2 * fc]])
                                P.act(SIL[fc][:], pG_, AF.Silu, r=[banks[0 + 2 * fc]])
                                P.tt(V, ACTT[ab][:, fc, :], SIL[fc][:], pU_, ALU.mult, r=[SIL[fc], banks[1 + 2 * fc]], w=[("ACTT%d" % ab, fc)])
                            for s in range(NSW):
                                j = tt_ * NSW + s
                                for dh in range(2):
                                    pb = 4 + (s % 2) * 2 + dh
                                    pD = banks[pb][:, :]
                                    for fc in range(2):
                                        P.mm(pD, ACTT[ab][:, fc, s * 128:(s + 1) * 128], WD[wb][:, fc, dh * 512:(dh + 1) * 512],
                                             start=(fc == 0), stop=(fc == 1), r=[("ACTT%d" % ab, 0), ("ACTT%d" % ab, 1), WD[wb]], w=[banks[pb]])
                                    accv = ACC[:, j, dh * 512:(dh + 1) * 512]
                                    if moe:
                                        P.stt(accv, pD, GATE[:, j, e:e + 1], accv, ALU.mult, ALU.add, r=[banks[pb], GATE, ("ACC", j)], w=[("ACC", j)])
                                    else:
                                        P.tt(V, accv, accv, pD, ALU.add, r=[banks[pb], ("ACC", j)], w=[("ACC", j)])
                for j in range(NJ):
                    ak = ("ACC", j)
                    row = g * GT + j * 128
                    if dst_is_final:
                        P.act(SQ[:], ACC[:, j, :], AF.Square, accum_out=SS[:, j:j + 1], r=[ak], w=[SQ, SS])
                        P.act(RS[:, j:j + 1], SS[:, j:j + 1], AF.Sqrt, bias=1e-6, scale=1.0 / D)
                        P.op(V, lambda e, j=j: e.reciprocal(out=RS[:, j:j + 1], in_=RS[:, j:j + 1]), [RS], [RS])
                        P.stt(ACC[:, j, :], ACC[:, j, :], RS[:, j:j + 1], GF[:], ALU.mult, ALU.mult, r=[ak, RS, GF], w=[ak])
                        out_ops.append(P.dma(SY, dst[row:row + 128, :], ACC[:, j, :], reads=[ak], writes=["out"]))
                    else:
                        jj = (src_row0 + row) // 128
                        P.ts(V, ACC[:, j, :], ACC[:, j, :], padmask[:, jj:jj + 1], None, ALU.mult, r=[ak, padmask], w=[ak])
                        P.dma(SY, dst[src_row0 + row:src_row0 + row + 128, :], ACC[:, j, :], reads=[ak], writes=["dst_ffn%d" % l])

    try:
        mixer(0, xs, hA, 0)
        P.barrier()
        chk("m0")
        ffn(0, hA, 0, NTOK, hB, False)
        P.barrier()
        chk("f0")
        mixer(1, hB, hA, NTILE // 2)
        P.barrier()
        chk("m1")
        ffn(1, hA, 0, NMOE, out_d, True)
    except _Stop:
        pass
    P.emit((SY, out_ops))
    return nc


def make_consts(TT=128):
    i = np.arange(64)[:, None]
    t = np.arange(64)[None, :]
    strict = (i < t).astype(np.float32)
    incl = (i <= t).astype(np.float32)
    maskar = np.concatenate([strict, incl], 1)
    masklow = (i > t).astype(np.float32)
    i8 = np.eye(64, dtype=np.float32)
    cmask = np.ones((128, TT), np.float32)
    cmask[:, ::64] = 0.0
    p = np.arange(128)
    blockones = (p[:, None] // 64 == p[None, :] // 64).astype(np.float32)
    headsel = np.zeros((128, 4, 8), np.float32)
    for hp in range(4):
        headsel[p, hp, 2 * hp + p // 64] = 1.0
    return {"c_ident": np.eye(128, dtype=np.float32), "c_maskar": np.ascontiguousarray(maskar),
            "c_masklow": np.ascontiguousarray(masklow), "c_i8": np.ascontiguousarray(i8), "c_cmask": cmask,
            "c_blockones": blockones, "c_headsel": headsel}


def make_core_inputs(x, NTOK, s):
    half = NTOK // 2
    wins = [2.0, 4.0, 8.0, 16.0]
    fixv = np.ones((128, 2, 16), np.float32)
    pos = np.arange(16) + 1.0
    for t in range(2):
        for hh in range(2):
            w = wins[2 * t + hh]
            fixv[hh * 64:(hh + 1) * 64, t, :] = w / np.minimum(pos, w)
    pfix = np.ones((128, 2, 2, 16), np.float32)
    if s == 1:
        xs = np.ascontiguousarray(x[:NTOK])
        mask = np.ones(NTOK, np.float32)
        pfix[:, :, 0, :] = fixv
    else:
        xs = np.concatenate([np.zeros((half, x.shape[1]), np.float32), x[:half]], 0)
        mask = np.concatenate([np.zeros(half, np.float32), np.ones(half, np.float32)])
        pfix[:, :, 1, :] = fixv
    padmask = np.ascontiguousarray(mask.reshape(NTOK // 128, 128).T)
    return {"xs": xs, "padmask": padmask, "pfix": pfix}


_WNAMES = ["norm1_g", "w_in", "pool_w", "pool_scale", "conv_w", "conv_b", "conv_ln_g", "conv_ln_b", "shift_mu",
           "rwkv_w0", "rwkv_w2", "rwkv_a0", "rwkv_a2", "rwkv_g2", "rwkv_k_k", "rwkv_k_a", "rwkv_r_k", "rwkv_gn_g",
           "rwkv_gn_b", "rwkv_v0", "rwkv_v1", "rwkv_v2", "w_out", "norm2_g", "ffn_w_gate", "ffn_w_up", "ffn_w_down",
           "moe_router", "moe_w_gate", "moe_w_up", "moe_w_down", "final_g"]


def run(inputs, NTOK=SEQ, GT=2048, TT=128):
    x = np.asarray(inputs["x"], np.float32)
    B = x.shape[0]
    nc = build(NTOK, GT, TT)
    consts = make_consts(TT)
    wts = {}
    for n in _WNAMES:
        a = np.ascontiguousarray(np.asarray(inputs[n], np.float32))
        if n == "rwkv_r_k":
            a = a.reshape(2, 512)
        if _KSTOP is not None and n in ("moe_w_gate", "moe_w_up"):
            a = np.ascontiguousarray(a[..., :8])
        if _KSTOP is not None and n == "moe_w_down":
            a = np.ascontiguousarray(a[:, :, :8, :])
        wts[n] = a
    in_maps = []
    for c in range(2 * B):
        b, s = c // 2, c % 2
        m = dict(wts)
        m.update(consts)
        m.update(make_core_inputs(x[b], NTOK, s))
        in_maps.append(m)
    res = run_bass_kernel_spmd(nc, in_maps, core_ids=list(range(2 * B)))
    half = NTOK // 2
    out = np.zeros((B, NTOK, D), np.float32)
    for c in range(2 * B):
        b, s = c // 2, c % 2
        out[b, s * half:(s + 1) * half] = res.results[c]["out"]
    return out


def kernel(**inputs):
    return run(inputs, SEQ, 2048, 128)
```

```python
from contextlib import ExitStack
import numpy as np
import concourse.bass as bass
import concourse.mybir as mybir
from concourse.bass_utils import run_bass_kernel_spmd

F32 = mybir.dt.float32
BF16 = mybir.dt.bfloat16
AF = mybir.ActivationFunctionType
ALU = mybir.AluOpType
AX = mybir.AxisListType

D = 1024
NIN = 2560
DFF = 2816
DFE = 3584
NE = 8
SEQ = 8192
NDMA_SLOTS = 8
_KSTOP = None
_KSKIP = ()


class _Stop(Exception):
    pass


def chk(tag):
    if _KSTOP == tag:
        raise _Stop()
V, A, G, T, SY = "vector", "scalar", "gpsimd", "tensor", "sync"
CDEC = 0.6065306597126334


class Prog:
    def __init__(self, nc):
        self.nc = nc
        self.ops = []
        self.last_write = {}
        self.readers = {}
        self.stacks = [ExitStack()]
        self.ntile = 0
        self.dnext = {SY: 0, A: 0, G: 0}
        self.last_on = {}
        self.bar = set()
        self.base = {}

    def scope(self):
        prog = self

        class _S:
            def __enter__(s):
                prog.stacks.append(ExitStack())

            def __exit__(s, *a):
                prog.barrier()
                prog.stacks.pop().close()
                return False
        return _S()

    def sb(self, shape, dtype, name):
        self.ntile += 1
        self.base[f"{name}_{self.ntile}"] = name
        return self.stacks[-1].enter_context(self.nc.sbuf_tensor(f"{name}_{self.ntile}", list(shape), dtype))

    def ps(self, shape, dtype, name):
        self.ntile += 1
        self.base[f"{name}_{self.ntile}"] = name
        return self.stacks[-1].enter_context(self.nc.psum_tensor(f"{name}_{self.ntile}", list(shape), dtype))

    def _k(self, x):
        if isinstance(x, (str, tuple)):
            return x
        return self.base.get(x.name, x.name)

    def barrier(self):
        self.bar = set(self.last_on.values())

    def op(self, eng, fn, reads=(), writes=(), dma=False):
        i = len(self.ops)
        deps = set(self.bar)
        reads = [self._k(x) for x in reads]
        writes = [self._k(x) for x in writes]
        if dma:
            slot = self.dnext[eng]
            self.dnext[eng] = (slot + 1) % NDMA_SLOTS
            semkey = (eng, slot)
        else:
            semkey = eng
        for k in reads:
            j = self.last_write.get(k)
            if j is not None:
                deps.add(j)
            if isinstance(k, str) and k.startswith("bank"):
                deps.update(v for sk, v in self.readers.get(k, {}).items() if sk != semkey)
        for k in writes:
            j = self.last_write.get(k)
            if j is not None:
                deps.add(j)
            deps.update(self.readers.get(k, {}).values())
        for k in reads:
            self.readers.setdefault(k, {})[semkey] = i
        for k in writes:
            self.last_write[k] = i
            self.readers[k] = {}
        self.last_on[semkey] = i
        self.ops.append((eng, fn, deps, semkey))
        return i

    def dma(self, q, out, in_, reads=None, writes=None, **kw):
        return self.op(q, lambda e: e.dma_start(out=out, in_=in_, **kw),
                       [in_] if reads is None else reads, [out] if writes is None else writes, dma=True)

    def tt(self, eng, out, in0, in1, op, r=None, w=None):
        return self.op(eng, lambda e: e.tensor_tensor(out=out, in0=in0, in1=in1, op=op),
                       [in0, in1] if r is None else r, [out] if w is None else w)

    def ts(self, eng, out, in0, s1, s2=None, op0=ALU.mult, op1=None, r=None, w=None):
        rr = [in0] + [x for x in (s1, s2) if not isinstance(x, (int, float, type(None)))]
        kw = {} if op1 is None else {"op1": op1}
        return self.op(eng, lambda e: e.tensor_scalar(out=out, in0=in0, scalar1=s1, scalar2=s2, op0=op0, **kw),
                       rr if r is None else r, [out] if w is None else w)

    def stt(self, out, in0, scalar, in1, op0, op1, r=None, w=None):
        rr = [in0, in1] + ([] if isinstance(scalar, (int, float)) else [scalar])
        return self.op(V, lambda e: e.scalar_tensor_tensor(out=out, in0=in0, scalar=scalar, in1=in1, op0=op0, op1=op1),
                       rr if r is None else r, [out] if w is None else w)

    def act(self, out, in_, func, bias=None, scale=1.0, accum_out=None, r=None, w=None):
        rr = [in_] + [x for x in (bias, scale) if not isinstance(x, (int, float, type(None)))]
        ww = [out] + ([accum_out] if accum_out is not None else [])
        kw = {}
        if bias is not None:
            kw["bias"] = bias
        if accum_out is not None:
            kw["accum_out"] = accum_out
        return self.op(A, lambda e: e.activation(out=out, in_=in_, func=func, scale=scale, **kw),
                       rr if r is None else r, ww if w is None else w)

    def cp(self, eng, out, in_, r=None, w=None):
        if eng == A:
            return self.act(out, in_, AF.Copy, r=r, w=w)
        return self.op(eng, lambda e: e.tensor_copy(out=out, in_=in_), [in_] if r is None else r, [out] if w is None else w)

    def mm(self, out, lhsT, rhs, start=True, stop=True, r=None, w=None):
        return self.op(T, lambda e: e.matmul(out, lhsT=lhsT, rhs=rhs, start=start, stop=stop),
                       [lhsT, rhs] if r is None else r, [out] if w is None else w)

    def tr(self, out, in_, ident, r=None, w=None):
        return self.op(T, lambda e: e.transpose(out=out, in_=in_, identity=ident),
                       [in_, ident] if r is None else r, [out] if w is None else w)

    def memset(self, eng, ap, val):
        return self.op(eng, lambda e: e.memset(ap, val), [], [ap])

    def emit(self, final_wait_ops):
        nc = self.nc
        engs = [SY, A, V, G, T]
        with ExitStack() as st:
            csem = {e: st.enter_context(nc.semaphore(f"c_{e}")) for e in engs if e != SY}
            dsem = {(e, i): st.enter_context(nc.semaphore(f"d_{e}{i}")) for e in (SY, A, G) for i in range(NDMA_SLOTS)}
            cnt = {}
            token = [None] * len(self.ops)
            prev = [None] * len(self.ops)
            per_eng = {e: [] for e in engs}
            for i, (eng, fn, deps, semkey) in enumerate(self.ops):
                c = cnt.get(semkey, 0)
                if isinstance(semkey, tuple):
                    prev[i] = (dsem[semkey], c * 16)
                    token[i] = (dsem[semkey], (c + 1) * 16)
                else:
                    token[i] = (csem[semkey], c + 1)
                cnt[semkey] = c + 1
                per_eng[eng].append(i)
            block = st.enter_context(nc.Block())
            ops = self.ops

            def body(engname):
                def run(e):
                    waited = {}
                    for i in per_eng[engname]:
                        eng, fn, deps, semkey = ops[i]
                        isdma = isinstance(semkey, tuple)
                        need = {}
                        for j in deps:
                            if engname == T and ops[j][3] == T:
                                continue
                            s, v = token[j]
                            if need.get(s.num, (None, 0))[1] < v:
                                need[s.num] = (s, v)
                        if isdma and prev[i][1] > 0:
                            s, v = prev[i]
                            if need.get(s.num, (None, 0))[1] < v:
                                need[s.num] = (s, v)
                        for s, v in need.values():
                            if waited.get(s.num, 0) < v:
                                e.wait_ge(s, v)
                                waited[s.num] = v
                        ins = fn(e)
                        ins.then_inc(token[i][0], 16 if isdma else 1)
                    if engname == final_wait_ops[0]:
                        for j in list(final_wait_ops[1]) + sorted(self.last_on.values()):
                            s, v = token[j]
                            if waited.get(s.num, 0) < v:
                                e.wait_ge(s, v)
                                waited[s.num] = v
                return run

            block.sync(body(SY))
            block.scalar(body(A))
            block.vector(body(V))
            block.gpsimd(body(G))
            block.tensor(body(T))
        self.stacks[0].close()


def build(NTOK=SEQ, GT=2048, TT=128):
    nc = bass.Bass("TRN2", target_bir_lowering=False)
    NMOE = NTOK // 2
    NCH = TT // 64
    NSUB = TT // 128
    NTILE = NTOK // TT

    def din(name, shape):
        return nc.dram_tensor(name, list(shape), F32, kind="ExternalInput").ap()

    xs = din("xs", [NTOK, D])
    padmask_d = din("padmask", [128, NTOK // 128])
    pfix_d = din("pfix", [128, 2, 2, 16])
    norm1_g = din("norm1_g", [2, D])
    w_in = din("w_in", [2, D, NIN])
    pool_w = din("pool_w", [2, 4, 64, 64])
    pool_scale = din("pool_scale", [2, 256])
    conv_w = din("conv_w", [2, 31, 256])
    conv_b = din("conv_b", [2, 256])
    conv_ln_g = din("conv_ln_g", [2, 256])
    conv_ln_b = din("conv_ln_b", [2, 256])
    shift_mu = din("shift_mu", [2, 1792])
    rwkv_w0 = din("rwkv_w0", [2, 512])
    rwkv_w2 = din("rwkv_w2", [2, 64, 512])
    rwkv_a0 = din("rwkv_a0", [2, 512])
    rwkv_a2 = din("rwkv_a2", [2, 64, 512])
    rwkv_g2 = din("rwkv_g2", [2, 128, 512])
    rwkv_k_k = din("rwkv_k_k", [2, 512])
    rwkv_k_a = din("rwkv_k_a", [2, 512])
    rwkv_r_k = din("rwkv_r_k", [2, 512])
    rwkv_gn_g = din("rwkv_gn_g", [2, 512])
    rwkv_gn_b = din("rwkv_gn_b", [2, 512])
    rwkv_v0 = din("rwkv_v0", [1, 512])
    rwkv_v1 = din("rwkv_v1", [1, 512, 32])
    rwkv_v2 = din("rwkv_v2", [1, 32, 512])
    w_out = din("w_out", [2, D, D])
    norm2_g = din("norm2_g", [2, D])
    ffn_w_gate = din("ffn_w_gate", [1, D, DFF])
    ffn_w_up = din("ffn_w_up", [1, D, DFF])
    ffn_w_down = din("ffn_w_down", [1, DFF, D])
    moe_router = din("moe_router", [1, D, NE])
    small = _KSTOP is not None
    moe_w_gate = din("moe_w_gate", [1, NE, D, 8 if small else DFE])
    moe_w_up = din("moe_w_up", [1, NE, D, 8 if small else DFE])
    moe_w_down = din("moe_w_down", [1, NE, 8 if small else DFE, D])
    final_g = din("final_g", [D])
    ident_d = din("c_ident", [128, 128])
    maskar_d = din("c_maskar", [64, 128])
    masklow_d = din("c_masklow", [64, 64])
    i8_d = din("c_i8", [64, 64])
    cmask_d = din("c_cmask", [128, TT])
    blockones_d = din("c_blockones", [128, 128])
    headsel_d = din("c_headsel", [128, 4, 8])
    out_d = nc.dram_tensor("out", [NMOE, D], F32, kind="ExternalOutput").ap()
    hA = nc.dram_tensor("hA", [NTOK, D], F32).ap()
    hB = nc.dram_tensor("hB", [NTOK, D], F32).ap()
    vfirst = nc.dram_tensor("vfirst", [512, NTOK], F32).ap()

    P = Prog(nc)
    out_ops = []

    identf = P.sb([128, 128], F32, "identf")
    identb = P.sb([128, 128], BF16, "identb")
    P.dma(SY, identf[:], ident_d)
    P.cp(V, identb[:], identf[:])
    padmask = P.sb([128, NTOK // 128], F32, "padmask")
    P.dma(SY, padmask[:], padmask_d)
    banks = [P.ps([128, 512], F32, f"bank{i}") for i in range(8)]

    def bview(bank_i, dtype, pattern=None, **kw):
        ap = banks[bank_i][:]
        if dtype is BF16:
            ap = ap.bitcast(BF16)
        if pattern:
            ap = ap.rearrange(pattern, **kw)
        return ap

    def mixer(l, src, dst, first_out_tile):
        with P.scope():
            WIN = P.sb([128, 8, NIN], BF16, "WIN")
            for c in range(8):
                for c0 in range(0, NIN, 640):
                    P.dma(G, WIN[:, c, c0:c0 + 640], w_in[l, c * 128:(c + 1) * 128, c0:c0 + 640], writes=[("WIN", c)])
            WINk = [("WIN", c) for c in range(8)]
            WOUT = P.sb([128, 8, D], BF16, "WOUT")
            for c in range(8):
                P.dma(G, WOUT[:, c, :], w_out[l, c * 128:(c + 1) * 128, :], writes=["WOUT"])
            G1 = P.sb([128, D], F32, "G1")
            P.dma(SY, G1[:], norm1_g[l].partition_broadcast(128))
            GNG = P.sb([64, 512], F32, "GNG")
            GNB = P.sb([64, 512], F32, "GNB")
            P.dma(SY, GNG[:], rwkv_gn_g[l].partition_broadcast(64))
            P.dma(SY, GNB[:], rwkv_gn_b[l].partition_broadcast(64))
            CMASK4 = P.sb([128, 4, TT], F32, "CMASK4")
            for h_ in range(4):
                P.dma(SY, CMASK4[:, h_, :], cmask_d, writes=[CMASK4])
            PV = P.sb([128, 64], F32, "PV")
            MU, OMU, W0, A0, KKc, KAc, OMKA, V0c, RKc, PSC, CB, LNG, LNB = 0, 14, 28, 32, 36, 40, 44, 48, 52, 56, 58, 60, 62
            CW = P.sb([128, 2, 31], F32, "CW")
            DIAG = P.sb([128, 2, 31, 128], BF16, "DIAG")
            PW = P.sb([128, 2, 128], BF16, "PW")
            with P.scope():
                PVR = P.sb([64, 128], F32, "PVR")
                P.memset(V, PVR[:], 0.0)

                def pvec(col, src_ap, n):
                    P.dma(SY, PVR[col:col + n, :], src_ap.rearrange("(c p) -> c p", p=128), writes=[PVR])
                pvec(MU, shift_mu[l], 14)
                pvec(W0, rwkv_w0[l], 4)
                pvec(A0, rwkv_a0[l], 4)
                pvec(KKc, rwkv_k_k[l], 4)
                pvec(KAc, rwkv_k_a[l], 4)
                if l == 1:
                    pvec(V0c, rwkv_v0[0], 4)
                pvec(RKc, rwkv_r_k[l], 4)
                pvec(PSC, pool_scale[l], 2)
                pvec(CB, conv_b[l], 2)
                pvec(LNG, conv_ln_g[l], 2)
                pvec(LNB, conv_ln_b[l], 2)
                P.tr(banks[0][:, 0:64], PVR[:], identf[0:64, 0:64])
                P.cp(V, PV[:], banks[0][:, 0:64])
                CWR = P.sb([31, 256], F32, "CWR")
                P.dma(SY, CWR[:], conv_w[l])
                for t in range(2):
                    P.tr(banks[1][:, t * 32:t * 32 + 31], CWR[:, t * 128:(t + 1) * 128], identf[0:31, 0:31])
                P.cp(V, CW[:], banks[1][:, 0:64].rearrange("p (t j) -> p t j", j=32)[:, :, 0:31])
                PWf = P.sb([128, 2, 128], F32, "PWf")
                P.memset(V, PWf[:], 0.0)
                for t in range(2):
                    P.dma(SY, PWf[0:64, t, 0:64], pool_w[l, 2 * t], writes=[PWf])
                    P.dma(SY, PWf[64:128, t, 64:128], pool_w[l, 2 * t + 1], writes=[PWf])
                P.cp(V, PW[:], PWf[:])
            P.ts(V, PV[:, OMU:OMU + 14], PV[:, MU:MU + 14], -1.0, 1.0, ALU.mult, ALU.add)
            P.ts(V, PV[:, OMKA:OMKA + 4], PV[:, KAc:KAc + 4], -1.0, 1.0, ALU.mult, ALU.add)
            for t in range(2):
                for j in range(31):
                    P.ts(V if j % 2 else G, DIAG[:, t, j, :], identf[:], CW[:, t, j:j + 1], None, ALU.mult)
            PFIX = P.sb([128, 2, 2, 16], F32, "PFIX")
            P.dma(SY, PFIX[:], pfix_d)
            W2A2 = P.sb([128, 512], BF16, "W2A2")
            P.dma(G, W2A2[0:64, :], rwkv_w2[l], writes=[W2A2])
            P.dma(G, W2A2[64:128, :], rwkv_a2[l], writes=[W2A2])
            G2 = P.sb([128, 512], BF16, "G2")
            P.dma(G, G2[:], rwkv_g2[l])
            if l == 1:
                V1 = P.sb([128, 4, 32], BF16, "V1")
                P.dma(G, V1[:], rwkv_v1[0].rearrange("(c p) r -> p c r", p=128))
                V2 = P.sb([32, 512], BF16, "V2")
                P.dma(G, V2[:], rwkv_v2[0])
            ONES256 = P.sb([128, 128], F32, "ONES256")
            P.memset(V, ONES256[:], 1.0 / 256.0)
            BLK = P.sb([128, 128], BF16, "BLK")
            HSEL = P.sb([128, 4, 8], BF16, "HSEL")
            with P.scope():
                BLKf = P.sb([128, 128], F32, "BLKf")
                P.dma(SY, BLKf[:], blockones_d)
                P.cp(V, BLK[:], BLKf[:])
                HSELf = P.sb([128, 4, 8], F32, "HSELf")
                P.dma(SY, HSELf[:], headsel_d)
                P.cp(V, HSEL[:], HSELf[:])
            MASKAR = P.sb([64, 128], F32, "MASKAR")
            MASKLOW = P.sb([64, 64], F32, "MASKLOW")
            I8 = P.sb([64, 64], F32, "I8")
            CMASK = P.sb([128, TT], F32, "CMASK")
            P.dma(SY, MASKAR[:], maskar_d)
            P.dma(SY, MASKLOW[:], masklow_d)
            P.dma(SY, I8[:], i8_d)
            P.dma(SY, CMASK[:], cmask_d)

            chk("m0w")
            def dbl(shape, dt, name):
                return [P.sb(shape, dt, f"{name}{b}") for b in range(2)]
            HXs = dbl([128, NSUB, D], F32, "HX")
            SQ = P.sb([128, D], BF16, "SQ")
            SS = P.sb([128, NSUB], F32, "SS")
            RS = P.sb([128, NSUB], F32, "RS")
            XN = P.sb([128, NSUB, D], BF16, "XN")
            HNT = P.sb([128, 8, TT], BF16, "HNT")
            UP = P.sb([128, 2, 16 + TT], F32, "UP")
            S1 = P.sb([128, 2, 16 + TT], F32, "S1")
            S2 = P.sb([128, 2, 16 + TT], F32, "S2")
            S3 = P.sb([128, 2, 16 + TT], F32, "S3")
            S16 = P.sb([128, 16 + TT], F32, "S16")
            DP = P.sb([128, 2, TT], BF16, "DP")
            CA = P.sb([128, 2, TT], F32, "CA")
            SG = P.sb([128, TT], F32, "SG")
            U = P.sb([128, 2, 32 + TT], BF16, "U")
            YB = P.sb([128, 2, TT], F32, "YB")
            YSQ = P.sb([128, 2, TT], F32, "YSQ")
            MEAN = P.sb([128, TT], F32, "MEAN")
            VAR = P.sb([128, TT], F32, "VAR")
            RSTD = P.sb([128, TT], F32, "RSTD")
            CT = P.sb([128, TT], F32, "CT")
            HALO = P.sb([128, 14], F32, "HALO")
            TMPM4 = P.sb([128, 4, TT], F32, "TMPM4")
            CT2 = P.sb([128, 2, TT], F32, "CT2")
            CF = P.sb([128, 12, TT], F32, "CF")
            ER = P.sb([128, 4, TT], F32, "ER")
            SGW4 = P.sb([128, 4, TT], F32, "SGW4")
            CUM4 = P.sb([128, 4, TT], F32, "CUM4")
            EA4 = P.sb([128, 4, TT], F32, "EA4")
            EI4 = P.sb([128, 4, TT], F32, "EI4")
            AA4 = P.sb([128, 4, TT], F32, "AA4")
            KK4 = P.sb([128, 4, TT], F32, "KK4")
            KSQ4 = P.sb([128, 4, TT], BF16, "KSQ4")
            RN4 = P.sb([128, 4, TT], F32, "RN4")
            KF4 = P.sb([128, 4, TT], F32, "KF4")
            BETA4 = P.sb([128, 4, TT], F32, "BETA4")
            VFT = P.sb([128, 4, TT], F32, "VFT") if l == 1 else None
            T1 = P.sb([32, TT], BF16, "T1") if l == 1 else None
            BH = P.sb([128, 4, TT], BF16, "BH")
            KH = P.sb([128, 4, TT], BF16, "KH")
            VB = P.sb([128, 4, TT], BF16, "VB")
            LACTs = dbl([128, 2, TT], BF16, "LACT")
            ARs = dbl([128, 4, NCH, 128], BF16, "AR")
            BTs = dbl([128, 4, TT], BF16, "BT")
            KTs = dbl([128, 4, TT], BF16, "KT")
            RKBs = dbl([128, 4, TT], BF16, "RKB")
            ARos = dbl([64, 4, NCH, 128], BF16, "ARo")
            BTos = dbl([64, 4, TT], BF16, "BTo")
            KTos = dbl([64, 4, TT], BF16, "KTo")
            ECos = dbl([64, 4, NCH], F32, "ECo")
            ECes = dbl([128, 4, NCH], F32, "ECe")
            VTOKs = dbl([64, NCH, 512], BF16, "VTOK")
            BHTs = dbl([64, NCH, 512], BF16, "BHT")
            KHTs = dbl([64, NCH, 512], BF16, "KHT")
            YMIXs = dbl([128, 8, TT], BF16, "YMIX")
            MBs = P.sb([64, 8, 128], BF16, "MBs")
            MKs = P.sb([64, 8, 128], BF16, "MKs")
            Mp = [P.sb([64, 8, 64], BF16, f"Mp{i}") for i in range(2)]
            Np = [P.sb([64, 8, 64], BF16, f"Np{i}") for i in range(2)]
            Tb = P.sb([64, 8, 64], BF16, "Tb")
            WTs = P.sb([64, 8, 64], BF16, "WTs")
            UTs = P.sb([64, 8, 64], BF16, "UTs")
            Sf = P.sb([64, 8, 64], F32, "Sf")
            Sb = P.sb([64, 8, 64], BF16, "Sb")
            YS = P.sb([64, 8, 64], F32, "YS")
            YQ = P.sb([64, 8, 64], F32, "YQ")
            ST = P.sb([64, 40], F32, "ST")
            BON = P.sb([64, 8], F32, "BON")
            YM = P.sb([64, 512], BF16, "YM")

            P.memset(V, UP[:], 0.0)
            P.memset(V, U[:], 0.0)
            P.memset(V, HALO[:], 0.0)
            P.memset(V, Sf[:], 0.0)
            P.memset(V, Sb[:], 0.0)
            for b_ in range(2):
                P.memset(G, ARs[b_][:], 0.0)
                P.memset(G, LACTs[b_][:], 0.0)

            def prep(it):
                b = it % 2
                t0 = it * TT
                lite = it < first_out_tile - 1
                HX, LACT, AR, BT, KT, RKB = HXs[b], LACTs[b], ARs[b], BTs[b], KTs[b], RKBs[b]
                ARo, BTo, KTo, ECo, ECe = ARos[b], BTos[b], KTos[b], ECos[b], ECes[b]
                VTOK, BHT, KHT, YMIX = VTOKs[b], BHTs[b], KHTs[b], YMIXs[b]
                ymn = "YMIX%d" % b
                arn, btn, ktn = "AR%d" % b, "BT%d" % b, "KT%d" % b
                P.dma(SY, HX[:], src[t0:t0 + TT, :].rearrange("(s p) d -> p s d", p=128))
                for s in range(NSUB):
                    P.act(SQ[:], HX[:, s, :], AF.Square, accum_out=SS[:, s:s + 1])
                    P.act(RS[:, s:s + 1], SS[:, s:s + 1], AF.Sqrt, bias=1e-6, scale=1.0 / D)
                    P.op(V, lambda e, s=s: e.reciprocal(out=RS[:, s:s + 1], in_=RS[:, s:s + 1]), [RS], [RS])
                    P.stt(XN[:, s, :], HX[:, s, :], RS[:, s:s + 1], G1[:], ALU.mult, ALU.mult)
                    yield
                    pT = bview(7, BF16, "p (a b) -> p a b", b=128)
                    for c in range(8):
                        P.tr(pT[:, c, :], XN[:, s, c * 128:(c + 1) * 128], identb[:], w=[banks[7]])
                    P.cp(V, HNT[:, :, s * 128:(s + 1) * 128], pT, r=[banks[7]])
                    yield

                def proj(f, pb):
                    pm = banks[pb][:, 0:TT]
                    for c in range(8):
                        P.mm(pm, WIN[:, c, f * 128:(f + 1) * 128], HNT[:, c, :], start=(c == 0), stop=(c == 7),
                             r=[("WIN", c), HNT], w=[banks[pb]])
                    return pm

                if not lite:
                    for t in range(2):
                        pm = proj(t, t % 2)
                        P.cp(A, UP[:, t, 16:16 + TT], pm, r=[banks[t % 2]], w=[UP])
                        yield
                    W_ = 16 + TT
                    P.tt(V, S1[:, :, 1:W_], UP[:, :, 1:W_], UP[:, :, 0:W_ - 1], ALU.add)
                    P.tt(G, S2[:, :, 3:W_], S1[:, :, 3:W_], S1[:, :, 1:W_ - 2], ALU.add)
                    yield
                    P.tt(V, S3[:, :, 7:W_], S2[:, :, 7:W_], S2[:, :, 3:W_ - 4], ALU.add)
                    P.tt(G, S16[:, 15:W_], S3[:, 1, 15:W_], S3[:, 1, 7:W_ - 8], ALU.add)
                    yield
                    grp = [(0, 0, S1[0:64, 0, :], 2.0), (0, 64, S2[64:128, 0, :], 4.0), (1, 0, S3[0:64, 1, :], 8.0), (1, 64, S16[64:128, :], 16.0)]
                    for (t, p0, sbuf, win) in grp:
                        for which, tile_idx in ((0, 0), (1, NTILE // 2)):
                            if it == tile_idx:
                                P.tt(V, sbuf[:, 16:32], sbuf[:, 16:32], PFIX[p0:p0 + 64, t, which, :], ALU.mult)
                        P.stt(DP[p0:p0 + 64, t, :], sbuf[:, 16:W_], 1.0 / win, UP[p0:p0 + 64, t, 16:W_], ALU.mult, ALU.subtract)
                    yield
                    P.cp(G, UP[:, :, 0:16], UP[:, :, TT:TT + 16])
                    for t in range(2):
                        pm2 = banks[t % 2][:, 0:TT]
                        P.mm(pm2, PW[:, t, :], DP[:, t, :], w=[banks[t % 2]])
                        P.act(YMIX[:, t, :], pm2, AF.Identity, scale=PV[:, PSC + t:PSC + t + 1], r=[banks[t % 2], PV], w=[(ymn, t)])
                    yield
                    for t in range(2):
                        pm = proj(2 + t, t % 2)
                        P.cp(A, CA[:, t, :], pm, r=[banks[t % 2]], w=[CA])
                        yield
                    for t in range(2):
                        pm = proj(4 + t, t % 2)
                        P.act(SG[:], pm, AF.Sigmoid, r=[banks[t % 2]])
                        P.tt(V, U[:, t, 32:32 + TT], CA[:, t, :], SG[:], ALU.mult)
                        yield
                    for t in range(2):
                        pc = banks[t % 2][:, 0:TT]
                        for j in range(31):
                            P.mm(pc, DIAG[:, t, j, :], U[:, t, 2 + j:2 + j + TT], start=(j == 0), stop=(j == 30), w=[banks[t % 2]])
                            if j % 8 == 7:
                                yield
                        P.act(YB[:, t, :], pc, AF.Identity, bias=PV[:, CB + t:CB + t + 1], r=[banks[t % 2], PV], w=[YB])
                        P.act(YSQ[:, t, :], pc, AF.Square, bias=PV[:, CB + t:CB + t + 1], r=[banks[t % 2], PV], w=[YSQ])
                        yield
                    P.cp(G, U[:, :, 0:32], U[:, :, TT:TT + 32])
                    pmean = banks[0][:, 0:TT]
                    psq = banks[1][:, 0:TT]
                    for t in range(2):
                        P.mm(pmean, ONES256[:], YB[:, t, :], start=(t == 0), stop=(t == 1), w=[banks[0]])
                    for t in range(2):
                        P.mm(psq, ONES256[:], YSQ[:, t, :], start=(t == 0), stop=(t == 1), w=[banks[1]])
                    P.cp(A, MEAN[:], pmean, r=[banks[0]])
                    P.act(VAR[:], pmean, AF.Square, r=[banks[0]])
                    yield
                    P.tt(V, VAR[:], psq, VAR[:], ALU.subtract, r=[banks[1], VAR])
                    P.act(RSTD[:], VAR[:], AF.Sqrt, bias=1e-5)
                    P.op(V, lambda e: e.reciprocal(out=RSTD[:], in_=RSTD[:]), [RSTD], [RSTD])
                    yield
                    for t in range(2):
                        P.tt(V, CT[:], YB[:, t, :], MEAN[:], ALU.subtract)
                        P.tt(V, CT[:], CT[:], RSTD[:], ALU.mult)
                        P.act(YMIX[:, 2 + t, :], CT[:], AF.Silu, bias=PV[:, LNB + t:LNB + t + 1], scale=PV[:, LNG + t:LNG + t + 1],
                              w=[(ymn, 2 + t)])
                        yield
                groups = [[4, 5, 6, 7], [8, 9, 10, 11], [12]] if lite else [[0, 1, 2, 3], [4, 5, 6, 7], [8, 9, 10, 11], [12, 13]]
                for gi, grp_ in enumerate(groups):
                    pb = gi % 2
                    n = len(grp_)
                    q0 = grp_[0]
                    pm4 = banks[pb][:, 0:4 * TT].rearrange("p (h t) -> p h t", t=TT)[:, 0:n, :]
                    for i_, q in enumerate(grp_):
                        for c in range(8):
                            P.mm(pm4[:, i_, :], WIN[:, c, (6 + q) * 128:(7 + q) * 128], HNT[:, c, :], start=(c == 0), stop=(c == 7),
                                 r=[("WIN", c), HNT], w=[banks[pb]])
                        if i_ % 2 == 1:
                            yield
                    mu4 = PV[:, MU + q0:MU + q0 + n].unsqueeze(2)
                    omu4 = PV[:, OMU + q0:OMU + q0 + n].unsqueeze(2)
                    hal = HALO[:, q0:q0 + n].unsqueeze(2)
                    P.tt(V, TMPM4[:, 0:n, 1:TT], pm4[:, :, 0:TT - 1], mu4.to_broadcast([128, n, TT - 1]), ALU.mult, r=[banks[pb], PV], w=[TMPM4])
                    P.tt(V, TMPM4[:, 0:n, 0:1], hal, mu4, ALU.mult, r=[HALO, PV], w=[TMPM4])
                    P.cp(A, hal, pm4[:, :, TT - 1:TT], r=[banks[pb]], w=[HALO])
                    if q0 < 12:
                        dstv = CF[:, q0:q0 + n, :]
                        wk = [("CF", q) for q in grp_]
                    else:
                        dstv = CT2[:, 0:n, :]
                        wk = [CT2]
                    P.tt(V, dstv, pm4, omu4.to_broadcast([128, n, TT]), ALU.mult, r=[banks[pb], PV], w=wk)
                    P.tt(V, dstv, dstv, TMPM4[:, 0:n, :], ALU.add, r=wk + [TMPM4], w=wk)
                    yield
                    if q0 == 12:
                        P.act(LACT[0:64, 0, :], CT2[0:64, 0, :], AF.Tanh, w=[LACT])
                        P.cp(V, LACT[64:128, 0, :], CT2[64:128, 0, :], w=[LACT])
                        if not lite:
                            P.act(LACT[:, 1, :], CT2[:, 1, :], AF.Sigmoid, w=[LACT])
                        yield
                CFr = [("CF", q) for q in range(12)]
                if l == 0:
                    P.dma(SY, vfirst[:, t0:t0 + TT].rearrange("(c p) t -> p c t", p=128), CF[:, 8:12, :], reads=CFr, writes=["vfirst"])
                    for hp in range(4):
                        P.cp(G, VB[:, hp, :], CF[:, 8 + hp, :], r=CFr, w=[("VB", hp)])
                    yield
                else:
                    P.dma(SY, VFT[:], vfirst[:, t0:t0 + TT].rearrange("(c p) t -> p c t", p=128), reads=["vfirst"], writes=[VFT])
                    for hp in range(4):
                        P.cp(G, VB[:, hp, :], CF[:, 8 + hp, :], r=CFr, w=[("VB", hp)])
                    yield
                    pv1 = banks[0][0:32, 0:TT]
                    for hp in range(4):
                        P.mm(pv1, V1[:, hp, :], VB[:, hp, :], start=(hp == 0), stop=(hp == 3), r=[V1] + [("VB", h) for h in range(4)], w=[banks[0]])
                    P.cp(A, T1[:], pv1, r=[banks[0]])
                    yield
                    for hp in range(4):
                        pv2 = banks[1][:, 0:TT]
                        P.mm(pv2, V2[0:32, hp * 128:(hp + 1) * 128], T1[:], w=[banks[1]])
                        P.act(SG[:], pv2, AF.Sigmoid, bias=PV[:, V0c + hp:V0c + hp + 1], r=[banks[1], PV])
                        P.tt(V, CT[:], VFT[:, hp, :], CF[:, 8 + hp, :], ALU.subtract, r=[VFT] + CFr)
                        P.tt(V, CT[:], CT[:], SG[:], ALU.mult)
                        P.tt(V, CF[:, 8 + hp, :], CF[:, 8 + hp, :], CT[:], ALU.add, r=CFr + [CT], w=[("CF", 8 + hp)])
                        yield
                    for hp in range(4):
                        P.cp(G, VB[:, hp, :], CF[:, 8 + hp, :], r=CFr, w=[("VB", hp)])
                    yield
                def flat(t_):
                    return t_[:].rearrange("p h t -> p (h t)")

                def c3(ap_):
                    return ap_.rearrange("p h (c t) -> p (h c) t", t=64)
                ERk = [("ER", h) for h in range(4)]
                pw4 = banks[0][:, 0:4 * TT].rearrange("p (h t) -> p h t", t=TT)
                pa4 = banks[1][:, 0:4 * TT].rearrange("p (h t) -> p h t", t=TT)
                for hp in range(4):
                    fs = slice(hp * 128, (hp + 1) * 128)
                    P.mm(pw4[:, hp, :], W2A2[0:64, fs], LACT[0:64, 0, :], w=[banks[0]])
                for hp in range(4):
                    fs = slice(hp * 128, (hp + 1) * 128)
                    P.mm(pa4[:, hp, :], W2A2[64:128, fs], LACT[64:128, 0, :], w=[banks[1]])
                yield
                for hp in range(4):
                    P.act(SGW4[:, hp, :], pw4[:, hp, :], AF.Sigmoid, bias=PV[:, W0 + hp:W0 + hp + 1], r=[banks[0], PV], w=[SGW4])
                yield
                for hp in range(4):
                    P.act(AA4[:, hp, :], pa4[:, hp, :], AF.Sigmoid, bias=PV[:, A0 + hp:A0 + hp + 1], r=[banks[1], PV], w=[AA4])
                yield
                P.op(V, lambda e: e.tensor_tensor_scan(out=flat(CUM4), data0=flat(CMASK4), data1=flat(SGW4), initial=0.0, op0=ALU.mult, op1=ALU.add),
                     [CMASK4, SGW4], [CUM4])
                P.tt(G, flat(EA4), flat(CUM4), flat(SGW4), ALU.subtract)
                P.act(flat(ER), flat(CUM4), AF.Exp, scale=-CDEC, r=[CUM4], w=ERk)
                yield
                P.act(flat(EA4), flat(EA4), AF.Exp, scale=-CDEC)
                P.act(flat(EI4), flat(CUM4), AF.Exp, scale=CDEC)
                kk4 = CF[:, 4:8, :]
                P.tt(V, KK4[:], kk4, PV[:, KKc:KKc + 4].unsqueeze(2).to_broadcast([128, 4, TT]), ALU.mult, r=CFr + [PV], w=[KK4])
                yield
                P.act(flat(KSQ4), flat(KK4), AF.Square)
                pss4 = banks[0][:, 0:4 * TT].rearrange("p (h t) -> p h t", t=TT)
                for hp in range(4):
                    P.mm(pss4[:, hp, :], BLK[:], KSQ4[:, hp, :], w=[banks[0]])
                P.act(flat(RN4), banks[0][:, 0:4 * TT], AF.Sqrt, bias=1e-24, r=[banks[0]], w=[RN4])
                EH4 = SGW4
                P.tt(V, c3(EH4[:]), c3(EI4[:]), c3(ER[:])[:, :, 63:64].to_broadcast([128, 4 * NCH, 64]), ALU.mult, r=[EI4] + ERk, w=[SGW4])
                yield
                P.op(V, lambda e: e.reciprocal(out=flat(RN4), in_=flat(RN4)), [RN4], [RN4])
                P.tt(V, flat(KK4), flat(KK4), flat(RN4), ALU.mult)
                TM24 = RN4
                P.tt(V, TM24[:], AA4[:], PV[:, KAc:KAc + 4].unsqueeze(2).to_broadcast([128, 4, TT]), ALU.mult, r=[AA4, PV, KK4], w=[RN4])
                P.tt(G, TM24[:], TM24[:], PV[:, OMKA:OMKA + 4].unsqueeze(2).to_broadcast([128, 4, TT]), ALU.add, r=[RN4, PV], w=[RN4])
                yield
                P.tt(V, KF4[:], kk4, TM24[:], ALU.mult, r=CFr + [RN4], w=[KF4])
                P.tt(G, flat(BETA4), flat(AA4), flat(KK4), ALU.mult)
                yield
                AR8 = AR[:].rearrange("p h c x -> p (h c) x")
                P.stt(AR8[:, :, 0:64], c3(KK4[:]), -1.0, c3(EA4[:]), ALU.mult, ALU.mult, r=[KK4, EA4], w=[AR])
                if not lite:
                    P.tt(V, AR8[:, :, 64:128], c3(CF[:, 0:4, :]), c3(ER[:]), ALU.mult, r=CFr + ERk, w=[AR])
                yield
                P.tt(V, flat(BT), flat(BETA4), flat(EI4), ALU.mult)
                P.tt(G, flat(KT), flat(KF4), flat(EI4), ALU.mult)
                yield
                P.tt(V, flat(BH), flat(BETA4), flat(EH4), ALU.mult, r=[BETA4, SGW4], w=[("BH", h) for h in range(4)])
                P.tt(G, flat(KH), flat(KF4), flat(EH4), ALU.mult, r=[KF4, SGW4], w=[("KH", h) for h in range(4)])
                if not lite:
                    P.tt(V, RKB[:], CF[:, 0:4, :], PV[:, RKc:RKc + 4].unsqueeze(2).to_broadcast([128, 4, TT]), ALU.mult, r=CFr + [PV], w=[RKB])
                    P.tt(V, flat(RKB), flat(RKB), flat(KF4), ALU.mult)
                yield
                ARk = [AR]
                BTk = [BT]
                KTk = [KT]
                P.dma(SY, ARo[:], AR[64:128, :, :, :], reads=ARk, writes=[ARo])
                P.dma(SY, BTo[:], BT[64:128, :, :], reads=BTk, writes=[BTo])
                P.dma(SY, KTo[:], KT[64:128, :, :], reads=KTk, writes=[KTo])
                ER4 = ER[:].rearrange("p h (c t) -> p h c t", t=64)
                P.cp(V, ECe[:], ER4[:, :, :, 63], r=[("ER", h) for h in range(4)], w=[ECe])
                P.dma(SY, ECo[:], ECe[64:128, :, :], reads=[ECe], writes=[ECo])
                yield
                for c in range(NCH):
                    cs = slice(c * 64, (c + 1) * 64)
                    for (srcT, dstT, kn) in ((VB, VTOK, "VB"), (BH, BHT, "BH"), (KH, KHT, "KH")):
                        pT = bview(7, BF16, "p (a b) -> p a b", b=128)
                        for hp in range(4):
                            P.tr(pT[0:64, hp, :], srcT[:, hp, cs], identb[:], r=[(kn, hp), identb], w=[banks[7]])
                        P.cp(A, dstT[:, c, :].rearrange("p (a b) -> p a b", b=128), pT[0:64, 0:4, :], r=[banks[7]], w=[dstT])
                        yield

            def scan(it):
                b = it % 2
                t0 = it * TT
                lite = it < first_out_tile - 1
                HX, LACT, AR, BT, KT, RKB = HXs[b], LACTs[b], ARs[b], BTs[b], KTs[b], RKBs[b]
                ARo, BTo, KTo, ECo, ECe = ARos[b], BTos[b], KTos[b], ECos[b], ECes[b]
                VTOK, BHT, KHT, YMIX = VTOKs[b], BHTs[b], KHTs[b], YMIXs[b]
                ymn = "YMIX%d" % b
                arn, btn, ktn = "AR%d" % b, "BT%d" % b, "KT%d" % b
                ARk = [AR]
                BTk = [BT]
                KTk = [KT]

                def fm(even, odd, h):
                    return (even[0:64, h // 2] if h % 2 == 0 else odd[0:64, h // 2])
                mar = MASKAR[:].unsqueeze(1).to_broadcast([64, 4, 128])
                for c in range(NCH):
                    cs = slice(c * 64, (c + 1) * 64)
                    rdk = ARk + BTk + KTk + [ARo, BTo, KTo]
                    pMa = bview(2, F32, "p (h x) -> p h x", x=128)
                    pMb = bview(3, F32, "p (h x) -> p h x", x=128)

                    def pM(h):
                        return (pMa if h < 4 else pMb)[0:64, h % 4, :]
                    pMk = [banks[2], banks[3]]
                    for h in range(8):
                        P.mm(pM(h), fm(BT, BTo, h)[:, cs], fm(AR, ARo, h)[:, c, :], r=rdk, w=[pMk[h // 4]])
                    P.tt(V, MBs[:, 0:4, :], pMa[0:64], mar, ALU.mult, r=[banks[2], MASKAR], w=[MBs])
                    P.tt(V, MBs[:, 4:8, :], pMb[0:64], mar, ALU.mult, r=[banks[3], MASKAR], w=[MBs])
                    yield
                    for h in range(8):
                        P.mm(pM(h), fm(KT, KTo, h)[:, cs], fm(AR, ARo, h)[:, c, :], r=rdk, w=[pMk[h // 4]])
                    P.tt(V, MKs[:, 0:4, :], pMa[0:64], mar, ALU.mult, r=[banks[2], MASKAR], w=[MKs])
                    P.tt(V, MKs[:, 4:8, :], pMb[0:64], mar, ALU.mult, r=[banks[3], MASKAR], w=[MKs])
                    yield
                    pC = bview(4, F32, "p (h x) -> p h x", x=64)
                    pC2 = bview(2, F32, "p (h x) -> p h x", x=64)
                    pC3 = bview(3, F32, "p (h x) -> p h x", x=64)
                    for h in range(8):
                        P.mm(pC[0:64, h, :], fm(AR, ARo, h)[:, c, 0:64], fm(BT, BTo, h)[:, cs], r=rdk, w=[banks[4]])
                    P.tt(V, Np[0][:], pC[0:64], MASKLOW[:].unsqueeze(1).to_broadcast([64, 8, 64]), ALU.mult, r=[banks[4], MASKLOW])
                    P.tt(V, Tb[:], MBs[:, :, 0:64], I8[:].unsqueeze(1).to_broadcast([64, 8, 64]), ALU.add, r=[MBs, I8])
                    yield
                    cur = 0
                    for step in range(5):
                        nxt = 1 - cur
                        last = (step == 4)
                        Mcur = (lambda h_: MBs[:, h_, 0:64]) if step == 0 else (lambda h_, cur=cur: Mp[cur][:, h_, :])
                        for h in range(8):
                            P.mm(pC[0:64, h, :], Mcur(h), Np[cur][:, h, :], w=[banks[4]])
                        P.cp(A, Np[nxt][:], pC[0:64], r=[banks[4]])
                        if not last:
                            for h in range(8):
                                P.mm(pC2[0:64, h, :], Np[cur][:, h, :], Mcur(h), w=[banks[2]])
                            P.cp(V, Mp[nxt][:], pC2[0:64], r=[banks[2]])
                        yield
                        for h in range(8):
                            P.mm(pC3[0:64, h, :], Np[nxt][:, h, :], Tb[:, h, :], w=[banks[3]])
                        P.tt(V, Tb[:], Tb[:], pC3[0:64], ALU.add, r=[Tb, banks[3]])
                        cur = nxt
                        yield
                    pZ = bview(5, F32, "p (h x) -> p h x", x=64)
                    for h in range(8):
                        hs = slice(h * 64, (h + 1) * 64)
                        P.mm(pZ[0:64, h, :], fm(AR, ARo, h)[:, c, 0:64], Sb[:, h, :], start=True, stop=False, r=rdk + [Sb], w=[banks[5]])
                        P.mm(pZ[0:64, h, :], MKs[:, h, 0:64], VTOK[:, c, hs], start=False, stop=True, w=[banks[5]])
                    P.cp(A, WTs[:], pZ[0:64], r=[banks[5]])
                    for par, ecsrc in ((0, ECe[0:64, :, c:c + 1]), (1, ECo[:, :, c:c + 1])):
                        Sv = Sf[:].rearrange("p (h two) x -> p h two x", two=2)[:, :, par, :]
                        P.tt(V, Sv, Sv, ecsrc.to_broadcast([64, 4, 64]), ALU.mult, r=[Sf, ECo, ECe], w=[Sf])
                    yield
                    for h in range(8):
                        P.mm(pZ[0:64, h, :], Tb[:, h, :], WTs[:, h, :], w=[banks[5]])
                    P.cp(V, UTs[:], pZ[0:64], r=[banks[5]])
                    yield
                    if not lite:
                        pY = bview(6, F32, "p (h x) -> p h x", x=64)
                        for h in range(8):
                            hs = slice(h * 64, (h + 1) * 64)
                            P.mm(pY[0:64, h, :], fm(AR, ARo, h)[:, c, 64:128], Sb[:, h, :], start=True, stop=False, r=rdk + [Sb], w=[banks[6]])
                            P.mm(pY[0:64, h, :], MBs[:, h, 64:128], UTs[:, h, :], start=False, stop=False, w=[banks[6]])
                            P.mm(pY[0:64, h, :], MKs[:, h, 64:128], VTOK[:, c, hs], start=False, stop=True, w=[banks[6]])
                        yield
                    for h in range(8):
                        hs = slice(h * 64, (h + 1) * 64)
                        P.mm(pZ[0:64, h, :], BHT[:, c, hs], UTs[:, h, :], start=True, stop=False, w=[banks[5]])
                        P.mm(pZ[0:64, h, :], KHT[:, c, hs], VTOK[:, c, hs], start=False, stop=True, w=[banks[5]])
                    P.tt(V, Sb[:], Sf[:], pZ[0:64], ALU.add, r=[Sf, banks[5]], w=[Sb])
                    P.tt(V, Sf[:], Sf[:], pZ[0:64], ALU.add, r=[Sf, banks[5]])
                    yield
                    if lite:
                        continue
                    P.cp(A, YS[:], pY[0:64], r=[banks[6]])
                    P.act(YQ[:], pY[0:64], AF.Square, r=[banks[6]])
                    P.op(V, lambda e: e.tensor_reduce(out=ST[:, 0:8], in_=YS[:], axis=AX.X, op=ALU.add), [YS], [ST])
                    P.op(V, lambda e: e.tensor_reduce(out=ST[:, 8:16], in_=YQ[:], axis=AX.X, op=ALU.add), [YQ], [ST])
                    yield
                    P.ts(V, ST[:, 16:24], ST[:, 0:8], 1.0 / 64, None, ALU.mult)
                    P.tt(V, ST[:, 24:32], ST[:, 16:24], ST[:, 16:24], ALU.mult)
                    P.stt(ST[:, 32:40], ST[:, 8:16], 1.0 / 64, ST[:, 24:32], ALU.mult, ALU.subtract)
                    P.act(ST[:, 32:40], ST[:, 32:40], AF.Sqrt, bias=64e-5)
                    P.op(V, lambda e: e.reciprocal(out=ST[:, 32:40], in_=ST[:, 32:40]), [ST], [ST])
                    yield
                    P.tt(V, YS[:], YS[:], ST[:, 16:24].unsqueeze(2).to_broadcast([64, 8, 64]), ALU.subtract, r=[YS, ST])
                    P.tt(V, YS[:], YS[:], ST[:, 32:40].unsqueeze(2).to_broadcast([64, 8, 64]), ALU.mult, r=[YS, ST])
                    YS2 = YS[:].rearrange("p h x -> p (h x)")
                    P.tt(V, YS2, YS2, GNG[:], ALU.mult, r=[YS, GNG], w=[YS])
                    P.tt(G, YS2, YS2, GNB[:], ALU.add, r=[YS, GNB], w=[YS])
                    yield
                    pBn = banks[3][0:64, 0:8]
                    for hp in range(4):
                        P.mm(pBn, RKB[:, hp, cs], HSEL[:, hp, :], start=(hp == 0), stop=(hp == 3), w=[banks[3]])
                    P.cp(A, BON[:], pBn, r=[banks[3]])
                    P.tt(V, YQ[:], VTOK[:, c, :].rearrange("p (h x) -> p h x", x=64), BON[:].unsqueeze(2).to_broadcast([64, 8, 64]), ALU.mult,
                         r=[VTOK, BON], w=[YQ])
                    P.tt(V, YS[:], YS[:], YQ[:], ALU.add)
                    yield
                    pG = banks[2][0:64, :]
                    P.mm(pG, LACT[:, 1, cs], G2[:], w=[banks[2]])
                    P.tt(V, YM[:], YS2, pG, ALU.mult, r=[YS, banks[2]], w=[YM])
                    pT2 = bview(4, BF16, "p (a b) -> p a b", b=128)
                    for hp in range(4):
                        P.tr(pT2[:, hp, 0:64], YM[:, hp * 128:(hp + 1) * 128], identb[0:64, 0:64], r=[YM, identb], w=[banks[4]])
                    P.cp(A, YMIX[:, 4:8, cs], pT2[:, 0:4, 0:64], r=[banks[4]], w=[(ymn, "rw")])
                    yield
                if it >= first_out_tile:
                    ymk = [(ymn, i) for i in (0, 1, 2, 3, "rw")]
                    for s in range(NSUB):
                        for dh in range(2):
                            po = banks[2 + dh][:, :]
                            for fc in range(8):
                                P.mm(po, YMIX[:, fc, s * 128:(s + 1) * 128], WOUT[:, fc, dh * 512:(dh + 1) * 512],
                                     start=(fc == 0), stop=(fc == 7), r=ymk + [WOUT], w=[banks[2 + dh]])
                            P.tt(V, HX[:, s, dh * 512:(dh + 1) * 512], HX[:, s, dh * 512:(dh + 1) * 512], po, ALU.add, r=[HX, banks[2 + dh]], w=[HX])
                            yield
                    d0 = t0 - first_out_tile * TT
                    P.dma(SY, dst[d0:d0 + TT, :].rearrange("(s p) d -> p s d", p=128), HX[:], reads=[HX], writes=["dst%d" % l])
                yield

            for it in range(NTILE + 1):
                gens = []
                if it < NTILE:
                    gens.append(prep(it))
                if it >= 1:
                    gens.append(scan(it - 1))
                while gens:
                    for g_ in list(gens):
                        try:
                            next(g_)
                        except StopIteration:
                            gens.remove(g_)
                chk("m0T%d" % it)

    def ffn(l, src, src_row0, ntok, dst, dst_is_final):
        moe = (l == 1)
        E = NE if moe else 1
        F = DFE if moe else DFF
        FG = 256
        NFG = F // FG
        NJ = GT // 128
        TW = min(512, GT)
        NTT = GT // TW
        NSW = TW // 128
        with P.scope():
            G2n = P.sb([128, D], F32, "G2n")
            P.dma(SY, G2n[:], norm2_g[l].partition_broadcast(128))
            if dst_is_final:
                GF = P.sb([128, D], F32, "GF")
                P.dma(SY, GF[:], final_g.partition_broadcast(128))
            if moe:
                RT = P.sb([128, 8, NE], F32, "RT")
                P.dma(SY, RT[:], moe_router[0].rearrange("(c p) e -> p c e", p=128))
                XNF = P.sb([128, D], F32, "XNF")
                XNTF = P.sb([128, 8, 128], F32, "XNTF")
                LOG = P.sb([128, 8], F32, "LOG")
                MX = P.sb([128, 8], F32, "MX")
                G12 = P.sb([128, 2], F32, "G12")
                EQ = P.sb([128, 8], F32, "EQ")
                GATE = P.sb([128, NJ, NE], F32, "GATE")
            ACC = P.sb([128, NJ, D], F32, "ACC")
            HNT = P.sb([128, 8, GT], BF16, "HNT2")
            SQ = P.sb([128, D], F32, "SQ2")
            SS = P.sb([128, NJ], F32, "SS2")
            RS = P.sb([128, NJ], F32, "RS2")
            XN = P.sb([128, D], BF16, "XN2")
            WG = [P.sb([128, 8, FG], BF16, f"WG{i}") for i in range(2)]
            WU = [P.sb([128, 8, FG], BF16, f"WU{i}") for i in range(2)]
            WD = [P.sb([128, 2, D], BF16, f"WD{i}") for i in range(2)]
            SIL = [P.sb([128, TW], F32, f"SIL{i}") for i in range(2)]
            ACTT = [P.sb([128, 2, TW], BF16, f"ACTT{i}") for i in range(2)]
            for g in range(ntok // GT):
                r0 = src_row0 + g * GT
                for j in range(NJ):
                    P.dma(SY, ACC[:, j, :], src[r0 + j * 128:r0 + (j + 1) * 128, :], reads=["src%d" % l], writes=[("ACC", j)])
                for j in range(NJ):
                    ak = ("ACC", j)
                    P.act(SQ[:], ACC[:, j, :], AF.Square, accum_out=SS[:, j:j + 1], r=[ak], w=[SQ, SS])
                    P.act(RS[:, j:j + 1], SS[:, j:j + 1], AF.Sqrt, bias=1e-6, scale=1.0 / D)
                    P.op(V, lambda e, j=j: e.reciprocal(out=RS[:, j:j + 1], in_=RS[:, j:j + 1]), [RS], [RS])
                    P.stt(XN[:], ACC[:, j, :], RS[:, j:j + 1], G2n[:], ALU.mult, ALU.mult, r=[ak, RS, G2n])
                    pT = bview(7, BF16, "p (a b) -> p a b", b=128)
                    for c in range(8):
                        P.tr(pT[:, c, :], XN[:, c * 128:(c + 1) * 128], identb[:], w=[banks[7]])
                    P.cp(V, HNT[:, :, j * 128:(j + 1) * 128], pT, r=[banks[7]], w=[("HNT2", j // NSW)])
                    if moe:
                        P.stt(XNF[:], ACC[:, j, :], RS[:, j:j + 1], G2n[:], ALU.mult, ALU.mult, r=[ak, RS, G2n])
                        for half in range(2):
                            pTf = bview(5 + half, F32, "p (a b) -> p a b", b=128)
                            for c4 in range(4):
                                c = half * 4 + c4
                                P.tr(pTf[:, c4, :], XNF[:, c * 128:(c + 1) * 128], identf[:], w=[banks[5 + half]])
                            P.cp(A, XNTF[:, half * 4:half * 4 + 4, :], pTf, r=[banks[5 + half]], w=[XNTF])
                        pR = banks[4][:, 0:NE]
                        for c in range(8):
                            P.mm(pR, XNTF[:, c, :], RT[:, c, :], start=(c == 0), stop=(c == 7), w=[banks[4]])
                        P.cp(A, LOG[:], pR, r=[banks[4]])
                        P.op(V, lambda e: e.max(out=MX[:], in_=LOG[:]), [LOG], [MX])
                        P.tt(V, G12[:, 0:1], MX[:, 0:1], MX[:, 1:2], ALU.subtract, w=[G12])
                        P.act(G12[:, 1:2], G12[:, 0:1], AF.Sigmoid, scale=-1.0)
                        P.act(G12[:, 0:1], G12[:, 0:1], AF.Sigmoid)
                        P.ts(V, EQ[:], LOG[:], MX[:, 0:1], G12[:, 0:1], ALU.is_equal, ALU.mult)
                        P.ts(V, GATE[:, j, :], LOG[:], MX[:, 1:2], G12[:, 1:2], ALU.is_equal, ALU.mult, w=[GATE])
                        P.tt(V, GATE[:, j, :], GATE[:, j, :], EQ[:], ALU.add, r=[GATE, EQ], w=[GATE])
                widx = 0
                for e in range(E):
                    wg_d = (moe_w_gate[0, e] if moe else ffn_w_gate[0])
                    wu_d = (moe_w_up[0, e] if moe else ffn_w_up[0])
                    wd_d = (moe_w_down[0, e] if moe else ffn_w_down[0])
                    for fg in range(NFG):
                        wb = widx % 2
                        widx += 1
                        f0 = fg * FG
                        P.dma(G, WG[wb][:], wg_d[:, f0:f0 + FG].rearrange("(c p) f -> p c f", p=128))
                        P.dma(G, WU[wb][:], wu_d[:, f0:f0 + FG].rearrange("(c p) f -> p c f", p=128))
                        P.dma(G, WD[wb][:], wd_d[f0:f0 + FG, :].rearrange("(c p) d -> p c d", p=128))
                        for tt_ in range(NTT):
                            ab = tt_ % 2
                            for fc in range(2):
                                pG_ = banks[0 + 2 * fc][:, 0:TW]
                                pU_ = banks[1 + 2 * fc][:, 0:TW]
                                for c in range(8):
                                    P.mm(pG_, WG[wb][:, c, fc * 128:(fc + 1) * 128], HNT[:, c, tt_ * TW:(tt_ + 1) * TW],
                                         start=(c == 0), stop=(c == 7), r=[WG[wb], ("HNT2", tt_)], w=[banks[0 + 2 * fc]])
                                for c in range(8):
                                    P.mm(pU_, WU[wb][:, c, fc * 128:(fc + 1) * 128], HNT[:, c, tt_ * TW:(tt_ + 1) * TW],
                                         start=(c == 0), stop=(c == 7), r=[WU[wb], ("HNT2", tt_)], w=[banks[1 + 2 * fc]])
                                P.act(SIL[fc][:], pG_, AF.Silu, r=[banks[0 + 2 * fc]])
                                P.tt(V, ACTT[ab][:, fc, :], SIL[fc][:], pU_, ALU.mult, r=[SIL[fc], banks[1 + 2 * fc]], w=[("ACTT%d" % ab, fc)])
                            for s in range(NSW):
                                j = tt_ * NSW + s
                                for dh in range(2):
                                    pb = 4 + (s % 2) * 2 + dh
                                    pD = banks[pb][:, :]
                                    for fc in range(2):
                                        P.mm(pD, ACTT[ab][:, fc, s * 128:(s + 1) * 128], WD[wb][:, fc, dh * 512:(dh + 1) * 512],
                                             start=(fc == 0), stop=(fc == 1), r=[("ACTT%d" % ab, 0), ("ACTT%d" % ab, 1), WD[wb]], w=[banks[pb]])
                                    accv = ACC[:, j, dh * 512:(dh + 1) * 512]
                                    if moe:
                                        P.stt(accv, pD, GATE[:, j, e:e + 1], accv, ALU.mult, ALU.add, r=[banks[pb], GATE, ("ACC", j)], w=[("ACC", j)])
                                    else:
                                        P.tt(V, accv, accv, pD, ALU.add, r=[banks[pb], ("ACC", j)], w=[("ACC", j)])
                for j in range(NJ):
                    ak = ("ACC", j)
                    row = g * GT + j * 128
                    if dst_is_final:
                        P.act(SQ[:], ACC[:, j, :], AF.Square, accum_out=SS[:, j:j + 1], r=[ak], w=[SQ, SS])
                        P.act(RS[:, j:j + 1], SS[:, j:j + 1], AF.Sqrt, bias=1e-6, scale=1.0 / D)
                        P.op(V, lambda e, j=j: e.reciprocal(out=RS[:, j:j + 1], in_=RS[:, j:j + 1]), [RS], [RS])
                        P.stt(ACC[:, j, :], ACC[:, j, :], RS[:, j:j + 1], GF[:], ALU.mult, ALU.mult, r=[ak, RS, GF], w=[ak])
                        out_ops.append(P.dma(SY, dst[row:row + 128, :], ACC[:, j, :], reads=[ak], writes=["out"]))
                    else:
                        jj = (src_row0 + row) // 128
                        P.ts(V, ACC[:, j, :], ACC[:, j, :], padmask[:, jj:jj + 1], None, ALU.mult, r=[ak, padmask], w=[ak])
                        P.dma(SY, dst[src_row0 + row:src_row0 + row + 128, :], ACC[:, j, :], reads=[ak], writes=["dst_ffn%d" % l])

    try:
        mixer(0, xs, hA, 0)
        P.barrier()
        chk("m0")
        ffn(0, hA, 0, NTOK, hB, False)
        P.barrier()
        chk("f0")
        mixer(1, hB, hA, NTILE // 2)
        P.barrier()
        chk("m1")
        ffn(1, hA, 0, NMOE, out_d, True)
    except _Stop:
        pass
    P.emit((SY, out_ops))
    return nc


def make_consts(TT=128):
    i = np.arange(64)[:, None]
    t = np.arange(64)[None, :]
    strict = (i < t).astype(np.float32)
    incl = (i <= t).astype(np.float32)
    maskar = np.concatenate([strict, incl], 1)
    masklow = (i > t).astype(np.float32)
    i8 = np.eye(64, dtype=np.float32)
    cmask = np.ones((128, TT), np.float32)
    cmask[:, ::64] = 0.0
    p = np.arange(128)
    blockones = (p[:, None] // 64 == p[None, :] // 64).astype(np.float32)
    headsel = np.zeros((128, 4, 8), np.float32)
    for hp in range(4):
        headsel[p, hp, 2 * hp + p // 64] = 1.0
    return {"c_ident": np.eye(128, dtype=np.float32), "c_maskar": np.ascontiguousarray(maskar),
            "c_masklow": np.ascontiguousarray(masklow), "c_i8": np.ascontiguousarray(i8), "c_cmask": cmask,
            "c_blockones": blockones, "c_headsel": headsel}


def make_core_inputs(x, NTOK, s):
    half = NTOK // 2
    wins = [2.0, 4.0, 8.0, 16.0]
    fixv = np.ones((128, 2, 16), np.float32)
    pos = np.arange(16) + 1.0
    for t in range(2):
        for hh in range(2):
            w = wins[2 * t + hh]
            fixv[hh * 64:(hh + 1) * 64, t, :] = w / np.minimum(pos, w)
    pfix = np.ones((128, 2, 2, 16), np.float32)
    if s == 1:
        xs = np.ascontiguousarray(x[:NTOK])
        mask = np.ones(NTOK, np.float32)
        pfix[:, :, 0, :] = fixv
    else:
        xs = np.concatenate([np.zeros((half, x.shape[1]), np.float32), x[:half]], 0)
        mask = np.concatenate([np.zeros(half, np.float32), np.ones(half, np.float32)])
        pfix[:, :, 1, :] = fixv
    padmask = np.ascontiguousarray(mask.reshape(NTOK // 128, 128).T)
    return {"xs": xs, "padmask": padmask, "pfix": pfix}


_WNAMES = ["norm1_g", "w_in", "pool_w", "pool_scale", "conv_w", "conv_b", "conv_ln_g", "conv_ln_b", "shift_mu",
           "rwkv_w0", "rwkv_w2", "rwkv_a0", "rwkv_a2", "rwkv_g2", "rwkv_k_k", "rwkv_k_a", "rwkv_r_k", "rwkv_gn_g",
           "rwkv_gn_b", "rwkv_v0", "rwkv_v1", "rwkv_v2", "w_out", "norm2_g", "ffn_w_gate", "ffn_w_up", "ffn_w_down",
           "moe_router", "moe_w_gate", "moe_w_up", "moe_w_down", "final_g"]


def run(inputs, NTOK=SEQ, GT=2048, TT=128):
    x = np.asarray(inputs["x"], np.float32)
    B = x.shape[0]
    nc = build(NTOK, GT, TT)
    consts = make_consts(TT)
    wts = {}
    for n in _WNAMES:
        a = np.ascontiguousarray(np.asarray(inputs[n], np.float32))
        if n == "rwkv_r_k":
            a = a.reshape(2, 512)
        if _KSTOP is not None and n in ("moe_w_gate", "moe_w_up"):
            a = np.ascontiguousarray(a[..., :8])
        if _KSTOP is not None and n == "moe_w_down":
            a = np.ascontiguousarray(a[:, :, :8, :])
        wts[n] = a
    in_maps = []
    for c in range(2 * B):
        b, s = c // 2, c % 2
        m = dict(wts)
        m.update(consts)
        m.update(make_core_inputs(x[b], NTOK, s))
        in_maps.append(m)
    res = run_bass_kernel_spmd(nc, in_maps, core_ids=list(range(2 * B)))
    half = NTOK // 2
    out = np.zeros((B, NTOK, D), np.float32)
    for c in range(2 * B):
        b, s = c // 2, c % 2
        out[b, s * half:(s + 1) * half] = res.results[c]["out"]
    return out


def kernel(**inputs):
    return run(inputs, SEQ, 2048, 128)
```
